# Optimizing a Trainium2 kernel written in Bass

```python
import jax, jax.numpy as jnp
from jax import lax
import numpy as np

D_MODEL = 1024
BATCH = 16
SEQ = 4096
DEPTH = 1

MLA_HEADS = 8
MLA_NOPE_DIM = 64
MLA_ROPE_DIM = 32
MLA_V_DIM = 64
MLA_Q_RANK = 256
MLA_KV_RANK = 128
MLA_WIDTH = MLA_HEADS * MLA_V_DIM
MOBA_HEADS = 8
MOBA_HEAD_DIM = 64
MOBA_WIDTH = MOBA_HEADS * MOBA_HEAD_DIM
MOBA_BLOCK = 256
MOBA_TOPK = 3
MOBA_Q_CHUNK = 16
ATTN_Q_BLOCK = 128
ROPE_THETA = 10000.0
EPS = 1e-6
N_BRANCHES = 2
IN_SIZES = (MLA_Q_RANK, MLA_KV_RANK, MLA_ROPE_DIM, MLA_WIDTH,
            MOBA_WIDTH, MOBA_WIDTH, MOBA_WIDTH, MOBA_WIDTH, N_BRANCHES * D_MODEL)
D_IN = MLA_Q_RANK + MLA_KV_RANK + MLA_ROPE_DIM + MLA_WIDTH + 4 * MOBA_WIDTH + N_BRANCHES * D_MODEL

kernel_name = "hybrid_mla_moba_gated_parallel"


def rms_norm(t, g):
    tf = t.astype(jnp.float32)
    tf = tf * lax.rsqrt(jnp.mean(tf * tf, axis=-1, keepdims=True) + EPS)
    return (tf * g.astype(jnp.float32)).astype(t.dtype)


def apply_rope(t, positions):
    half = t.shape[-1] // 2
    inv_freq = ROPE_THETA ** (-jnp.arange(half, dtype=jnp.float32) / half)
    ang = positions.astype(jnp.float32)[:, :, None, None] * inv_freq
    cos, sin = jnp.cos(ang), jnp.sin(ang)
    tf = t.astype(jnp.float32)
    t1, t2 = tf[..., :half], tf[..., half:]
    return jnp.concatenate([t1 * cos - t2 * sin, t2 * cos + t1 * sin], axis=-1).astype(t.dtype)


def split_cols(t, sizes):
    outs, start = [], 0
    for n in sizes:
        outs.append(t[..., start:start + n])
        start += n
    return outs


def causal_dense_attention(q, k, v, scale):
    B, S, H, _ = q.shape
    kpos = jnp.arange(S)

    def one_block(i):
        start = i * ATTN_Q_BLOCK
        qb = lax.dynamic_slice_in_dim(q, start, ATTN_Q_BLOCK, axis=1)
        s = jnp.einsum('bqhd,bkhd->bhqk', qb, k).astype(jnp.float32) * scale
        qpos = start + jnp.arange(ATTN_Q_BLOCK)
        s = jnp.where(kpos[None, :] <= qpos[:, None], s, -jnp.inf)
        p = jax.nn.softmax(s, axis=-1).astype(v.dtype)
        return jnp.einsum('bhqk,bkhd->bqhd', p, v)

    out = lax.map(one_block, jnp.arange(S // ATTN_Q_BLOCK))
    return out.transpose(1, 0, 2, 3, 4).reshape(B, S, H, v.shape[-1])


def moba_attention(q, k, v):
    B, S, H, dh = q.shape
    nb = -(-S // MOBA_BLOCK)
    n_top = min(MOBA_TOPK, nb)
    pad = nb * MOBA_BLOCK - S
    kp = jnp.pad(k, ((0, 0), (0, pad), (0, 0), (0, 0)))
    vp = jnp.pad(v, ((0, 0), (0, pad), (0, 0), (0, 0)))
    kb = kp.reshape(B, nb, MOBA_BLOCK, H, dh).transpose(0, 3, 1, 2, 4)
    vb = vp.reshape(B, nb, MOBA_BLOCK, H, dh).transpose(0, 3, 1, 2, 4)
    kmean = jnp.mean(kb.astype(jnp.float32), axis=3)
    scale = dh ** -0.5
    bi = jnp.arange(B)[:, None, None, None]
    hi = jnp.arange(H)[None, :, None, None]
    in_blk = jnp.arange(MOBA_BLOCK)
    blk_ids = jnp.arange(nb)
    n_sel = n_top * MOBA_BLOCK

    def one_chunk(i):
        start = i * MOBA_Q_CHUNK
        qc = lax.dynamic_slice_in_dim(q, start, MOBA_Q_CHUNK, axis=1)
        qpos = start + jnp.arange(MOBA_Q_CHUNK)
        own = start // MOBA_BLOCK
        gate = jnp.einsum('bqhd,bhnd->bhqn', qc.astype(jnp.float32), kmean)
        gate = jnp.where(blk_ids < own, gate, -jnp.inf)
        _, idx = lax.top_k(gate, n_top)
        valid = jnp.arange(n_top) < own
        k_sel = kb[bi, hi, idx]
        v_sel = vb[bi, hi, idx]
        s_sel = jnp.einsum('bqhd,bhqnkd->bhqnk', qc, k_sel).astype(jnp.float32) * scale
        s_sel = jnp.where(valid[:, None], s_sel, -jnp.inf)
        k_own = lax.dynamic_index_in_dim(kb, own, axis=2, keepdims=False)
        v_own = lax.dynamic_index_in_dim(vb, own, axis=2, keepdims=False)
        s_own = jnp.einsum('bqhd,bhkd->bhqk', qc, k_own).astype(jnp.float32) * scale
        kpos_own = own * MOBA_BLOCK + in_blk
        s_own = jnp.where(kpos_own[None, :] <= qpos[:, None], s_own, -jnp.inf)
        s = jnp.concatenate([s_sel.reshape(B, H, MOBA_Q_CHUNK, n_sel), s_own], axis=-1)
        p = jax.nn.softmax(s, axis=-1).astype(v.dtype)
        p_sel = p[..., :n_sel].reshape(B, H, MOBA_Q_CHUNK, n_top, MOBA_BLOCK)
        p_own = p[..., n_sel:]
        return (jnp.einsum('bhqnk,bhqnkd->bqhd', p_sel, v_sel)
                + jnp.einsum('bhqk,bhkd->bqhd', p_own, v_own))

    out = lax.map(one_chunk, jnp.arange(S // MOBA_Q_CHUNK))
    return out.transpose(1, 0, 2, 3, 4).reshape(B, S, H, dh)


def setup_inputs(seed: int = 0) -> dict:
    key = jax.random.key(seed)
    ks = jax.random.split(key, 20)

    def w(k, shape, fan_in, mult=1.0):
        return jax.random.normal(k, shape, jnp.float32) * (mult * fan_in ** -0.5)

    def gain(k, n):
        return 1.0 + 0.05 * jax.random.normal(k, (DEPTH, n), jnp.float32)

    x = jax.random.normal(ks[0], (BATCH, SEQ, D_MODEL), jnp.float32)
    c = jax.random.normal(ks[1], (BATCH, D_MODEL), jnp.float32)
    offset = jax.random.randint(ks[2], (BATCH, 1), 0, 1024, dtype=jnp.int32)
    positions = (offset + jnp.arange(SEQ, dtype=jnp.int32)[None, :]).astype(jnp.int32)
    return {
        "x": x,
        "c": c,
        "positions": positions,
        "w_ada": w(ks[3], (DEPTH, D_MODEL, 3 * D_MODEL), D_MODEL, 0.5),
        "b_ada": 0.01 * jax.random.normal(ks[4], (DEPTH, 3 * D_MODEL), jnp.float32),
        "g_pre": gain(ks[5], D_MODEL),
        "g_post": gain(ks[6], D_MODEL),
        "w_in": w(ks[7], (DEPTH, D_MODEL, D_IN), D_MODEL),
        "g_q_lat": gain(ks[8], MLA_Q_RANK),
        "w_uq": w(ks[9], (DEPTH, MLA_Q_RANK, MLA_HEADS * (MLA_NOPE_DIM + MLA_ROPE_DIM)), MLA_Q_RANK),
        "g_kv_lat": gain(ks[10], MLA_KV_RANK),
        "w_ukv": w(ks[11], (DEPTH, MLA_KV_RANK, MLA_HEADS * (MLA_NOPE_DIM + MLA_V_DIM)), MLA_KV_RANK),
        "w_o_mla": w(ks[12], (DEPTH, MLA_WIDTH, D_MODEL), MLA_WIDTH),
        "w_o_moba": w(ks[13], (DEPTH, MOBA_WIDTH, D_MODEL), MOBA_WIDTH),
        "b_merge": 0.01 * jax.random.normal(ks[14], (DEPTH, N_BRANCHES * D_MODEL), jnp.float32),
        "w_out": w(ks[15], (DEPTH, D_MODEL, D_MODEL), D_MODEL),
    }


def reference(x, c, positions, w_ada, b_ada, g_pre, g_post, w_in, g_q_lat, w_uq,
              g_kv_lat, w_ukv, w_o_mla, w_o_moba, b_merge, w_out):
    B, S, D = x.shape
    for l in range(DEPTH):
        mod = jax.nn.silu(c) @ w_ada[l] + b_ada[l]
        shift, scale, gate = jnp.split(mod, 3, axis=-1)
        h = rms_norm(x, g_pre[l]) * (1 + scale[:, None, :]) + shift[:, None, :]

        proj = h @ w_in[l]
        (q_lat, kv_lat, k_rope, z_mla, q_mb, k_mb, v_mb, z_mb,
         merge_logits) = split_cols(proj, IN_SIZES)

        q = (rms_norm(q_lat, g_q_lat[l]) @ w_uq[l]).reshape(B, S, MLA_HEADS, MLA_NOPE_DIM + MLA_ROPE_DIM)
        q_nope, q_pe = q[..., :MLA_NOPE_DIM], q[..., MLA_NOPE_DIM:]
        kv = (rms_norm(kv_lat, g_kv_lat[l]) @ w_ukv[l]).reshape(B, S, MLA_HEADS, MLA_NOPE_DIM + MLA_V_DIM)
        k_nope, v_mla = kv[..., :MLA_NOPE_DIM], kv[..., MLA_NOPE_DIM:]
        q_pe = apply_rope(q_pe, positions)
        k_pe = apply_rope(k_rope[:, :, None, :], positions)
        q_full = jnp.concatenate([q_nope, q_pe], axis=-1)
        k_full = jnp.concatenate([k_nope, jnp.broadcast_to(k_pe, (B, S, MLA_HEADS, MLA_ROPE_DIM))], axis=-1)
        o_mla = causal_dense_attention(q_full, k_full, v_mla,
                                       (MLA_NOPE_DIM + MLA_ROPE_DIM) ** -0.5).reshape(B, S, MLA_WIDTH)
        y_mla = (o_mla * jax.nn.silu(z_mla)) @ w_o_mla[l]

        qm = apply_rope(q_mb.reshape(B, S, MOBA_HEADS, MOBA_HEAD_DIM), positions)
        km = apply_rope(k_mb.reshape(B, S, MOBA_HEADS, MOBA_HEAD_DIM), positions)
        vm = v_mb.reshape(B, S, MOBA_HEADS, MOBA_HEAD_DIM)
        o_mb = moba_attention(qm, km, vm).reshape(B, S, MOBA_WIDTH)
        y_mb = (o_mb * jax.nn.silu(z_mb)) @ w_o_moba[l]

        gates = jax.nn.sigmoid(merge_logits + b_merge[l])
        g_a, g_b = jnp.split(gates, 2, axis=-1)
        y = (g_a * y_mla + g_b * y_mb) @ w_out[l]

        x = x + gate[:, None, :] * rms_norm(y, g_post[l])
    return x
```

```python
import numpy as np
import ml_dtypes
import concourse.bass as bass
import concourse.mybir as mybir
from concourse.bass_utils import run_bass_kernel_spmd

F32 = mybir.dt.float32
BF16 = mybir.dt.bfloat16
I32 = mybir.dt.int32
ALU = mybir.AluOpType
AF = mybir.ActivationFunctionType
AX = mybir.AxisListType

NCORES = 8
SEQ = 4096
DM = 1024
D_IN = 5024
NCH = 8
NT = 32
EPS = 1e-6
NEG = -30000.0
TWO_PI = 6.283185307179586
PI = 3.141592653589793

C_QLAT, C_KVLAT, C_KROPE, C_ZMLA, C_QMB, C_KMB, C_VMB, C_ZMB, C_MERGE = 0, 256, 384, 416, 928, 1440, 1952, 2464, 2976


class Sched:
    ENGS = ("pe", "act", "dve", "pool", "sp")
    PER_SEM = 30000

    NDMASEM = 16

    def __init__(self):
        self.ops = []
        self.last_w = {}
        self.readers = {}
        self.dma_ops = {}
        self.last_of_stream = {}
        self.fence_deps = set()
        self.fence_pending = set()

    def add(self, eng, fn, r=(), w=(), dma=False):
        i = len(self.ops)
        deps = set()
        for res in r:
            j = self.last_w.get(res)
            if j is not None:
                deps.add(j)
            if isinstance(res, tuple) and res[0] == "ps":
                for j in self.readers.get(res, ()):
                    if self.ops[j]["eng"] != eng:
                        deps.add(j)
        for res in w:
            j = self.last_w.get(res)
            if j is not None:
                deps.add(j)
            for j in self.readers.get(res, ()):
                deps.add(j)
        for res in r:
            self.readers.setdefault(res, []).append(i)
        for res in w:
            self.last_w[res] = i
            self.readers[res] = []
        deps.discard(i)
        dman = None
        if dma:
            lst = self.dma_ops.setdefault(eng, [])
            dman = len(lst)
            if dman >= self.NDMASEM:
                deps.add(lst[dman - self.NDMASEM])
            lst.append(i)
        if eng in self.fence_pending:
            deps |= self.fence_deps
            self.fence_pending.discard(eng)
        self.ops.append(dict(eng=eng, fn=fn, deps=deps, dma=dma, dman=dman))
        self.last_of_stream[self.stream_of(i)] = i
        return i

    def fence(self):
        self.fence_deps = set(self.last_of_stream.values())
        self.fence_pending = set(self.ENGS)

    def stream_of(self, i):
        o = self.ops[i]
        return ("dma", o["eng"], o["dman"] % self.NDMASEM) if o["dma"] else ("eng", o["eng"])

    def finalize(self):
        waited = {}
        for i, o in enumerate(self.ops):
            best = {}
            for j in o["deps"]:
                s = self.stream_of(j)
                if s == ("eng", "pe") and o["eng"] == "pe" and not o["dma"]:
                    continue
                if j > best.get(s, -1):
                    best[s] = j
            waits = []
            for s, j in best.items():
                key = (o["eng"], s)
                if waited.get(key, -1) >= j:
                    continue
                waited[key] = j
                waits.append(j)
            o["waits"] = waits
        signal = set()
        for o in self.ops:
            signal.update(o["waits"])
        counts = {}
        for i, o in enumerate(self.ops):
            if o["dma"]:
                s = self.stream_of(i)
                n = o["dman"] // self.NDMASEM + 1
                counts[s] = n
                o["sig"] = (s, n)
            elif i in signal:
                s = self.stream_of(i)
                n = counts.get(s, 0) + 1
                counts[s] = n
                o["sig"] = (s, n)
            else:
                o["sig"] = None
        self.counts = counts

    def emit(self, nc, block_ctx, sems):
        def sem_for(s, n):
            if s[0] == "dma":
                return sems[s][0], n * 16
            k = (n - 1) // self.PER_SEM
            return sems[s][k], ((n - 1) % self.PER_SEM) + 1

        def run(engname, e):
            for o in self.ops:
                if o["eng"] != engname:
                    continue
                for j in o["waits"]:
                    s, n = self.ops[j]["sig"]
                    sh, val = sem_for(s, n)
                    e.wait_ge(sh, val)
                if o["fn"] is None:
                    continue
                ins = o["fn"](e)
                if o["sig"] is not None:
                    s, n = o["sig"]
                    if s[0] == "dma":
                        ins.then_inc(sems[s][0], 16)
                    else:
                        ins.then_inc(sems[s][(n - 1) // self.PER_SEM], 1)

        @block_ctx.tensor
        def _(e):
            run("pe", e)

        @block_ctx.scalar
        def _(e):
            run("act", e)

        @block_ctx.vector
        def _(e):
            run("dve", e)

        @block_ctx.gpsimd
        def _(e):
            run("pool", e)

        @block_ctx.sync
        def _(e):
            run("sp", e)


def _consts():
    bf = ml_dtypes.bfloat16
    c = {}
    c["ident_f"] = np.eye(128, dtype=np.float32)
    c["ident_b"] = np.eye(128, dtype=np.float32).astype(bf)
    k = np.arange(128)[:, None]
    q = np.arange(128)[None, :]
    c["tri_b"] = np.where(k <= q, 0.0, NEG).astype(np.float32).astype(bf)
    rm64 = np.zeros((128, 128), np.float32)
    for base in (0, 64):
        for m in range(64):
            if m < 32:
                rm64[base + m + 32, base + m] = -1.0
            else:
                rm64[base + m - 32, base + m] = 1.0
    c["rm64"] = rm64.astype(bf)
    rmm = np.zeros((128, 128), np.float32)
    for base in (0, 32, 64, 96):
        for m in range(32):
            if m < 16:
                rmm[base + m + 16, base + m] = -1.0
            else:
                rmm[base + m - 16, base + m] = 1.0
    c["rmmla"] = rmm.astype(bf)
    p = np.arange(128)
    invf = np.zeros((128, 2), np.float32)
    f0 = (np.float32(10000.0) ** (-((p % 64) % 32).astype(np.float32) / np.float32(32))).astype(np.float32)
    f1 = (np.float32(10000.0) ** (-((p % 32) % 16).astype(np.float32) / np.float32(16))).astype(np.float32)
    invf[:, 0] = (f0.astype(np.float64) / (2 * np.pi)).astype(np.float32)
    invf[:, 1] = (f1.astype(np.float64) / (2 * np.pi)).astype(np.float32)
    c["invf"] = invf
    cb = np.zeros((32, 16), np.float32)
    for qt in range(32):
        own = qt // 2
        for n in range(16):
            cb[qt, n] = 0.0 if n < own else (1e30 if n == own else -1e30)
    c["cb"] = np.ascontiguousarray(np.broadcast_to(cb.reshape(1, 512), (128, 512))).astype(np.float32)
    eind = np.zeros((16, SEQ), np.float32)
    for n in range(16):
        eind[n, n * 256:(n + 1) * 256] = 1.0
    c["eind"] = eind.astype(bf)
    return c


def _T(v, ntile):
    return np.ascontiguousarray(np.asarray(v, np.float32).reshape(ntile, 128).T)


def build(NS=2, debug=None):
    nc = bass.Bass("TRN2", target_bir_lowering=False)
    S = Sched()
    dbg_out = {}

    def din(name, shape, dt=F32):
        return nc.dram_tensor(name, list(shape), dt, kind="ExternalInput").ap()

    x_d = din("x", [NS, SEQ, DM])
    pos_d = din("pos", [NS, SEQ], I32)
    cT_d = din("cT", [128, 8 * NS])
    wada_d = din("w_ada", [DM, 3 * DM])
    badaT_d = din("badaT", [128, 24])
    badag_d = din("badag_bc", [128, DM])
    gpreT_d = din("gpreT", [128, 8])
    gpost_d = din("gpost_bc", [128, DM])
    win_d = din("w_in", [DM, D_IN])
    gqT_d = din("gqT", [128, 2])
    gkvT_d = din("gkvT", [128, 1])
    wuq_d = din("w_uq", [256, 768])
    wukv_d = din("w_ukv", [128, 1024])
    womla_d = din("w_o_mla", [512, DM])
    womb_d = din("w_o_mb", [512, DM])
    bmT_d = din("bmT", [128, 16])
    wout_d = din("w_out", [DM, DM])
    identf_d = din("ident_f", [128, 128])
    identb_d = din("ident_b", [128, 128], BF16)
    tri_d = din("tri_b", [128, 128], BF16)
    rm64_d = din("rm64", [128, 128], BF16)
    rmmla_d = din("rmmla", [128, 128], BF16)
    invf_d = din("invf", [128, 2])
    cb_d = din("cb", [128, 512])
    eind_d = din("eind", [16, SEQ], BF16)
    out_d = nc.dram_tensor("out", [NS, SEQ, DM], F32, kind="ExternalOutput").ap()
    ogscr_d = nc.dram_tensor("og_scr", [2, 4, 128, SEQ], BF16, kind="Internal").ap()

    def dbg(name, shape, dt=F32):
        t = nc.dram_tensor("dbg_" + name, list(shape), dt, kind="ExternalOutput").ap()
        dbg_out[name] = t
        return t

    ARENA_BYTES = 210000
    from contextlib import ExitStack
    es = ExitStack()
    arena = es.enter_context(nc.sbuf_tensor("arena", [128, ARENA_BYTES // 2], BF16))
    ps = [es.enter_context(nc.psum_tensor("ps%d" % i, [128, 512], F32)) for i in range(8)]

    def carve(off, shape, dt):
        esz = 4 if dt in (F32, I32) else 2
        n = int(np.prod(shape))
        assert off % 4 == 0
        a = arena[:, off // 2: off // 2 + n * esz // 2]
        if dt != BF16:
            a = a.bitcast(dt)
        if len(shape) == 2:
            a = a.rearrange("p (a b) -> p a b", a=shape[0])
        elif len(shape) == 3:
            a = a.rearrange("p (a b c) -> p a b c", a=shape[0], b=shape[1])
        return a, off + n * esz

    class Alloc:
        def __init__(self, start, limit):
            self.off = start
            self.limit = limit

        def get(self, shape, dt):
            if isinstance(shape, int):
                shape = [shape]
            a, self.off = carve(self.off, shape, dt)
            self.off = (self.off + 31) // 32 * 32
            assert self.off <= self.limit, (self.off, self.limit)
            return a

    PA = Alloc(0, 82000)
    hT = PA.get([8, SEQ], BF16)
    ident_f = PA.get(128, F32)
    ident_b = PA.get(128, BF16)
    tri_b = PA.get(128, BF16)
    rm64 = PA.get(128, BF16)
    rmmla = PA.get(128, BF16)
    ones_b = PA.get(128, BF16)
    half_b = PA.get(128, BF16)
    cb = PA.get([32, 16], F32)
    invf = PA.get(2, F32)
    A_sb = PA.get([NS, 8], F32)
    B_sb = PA.get([NS, 8], F32)
    G2 = PA.get([NS, DM], F32)
    gqT = PA.get(2, F32)
    gkvT = PA.get(1, F32)
    bmh = PA.get(16, F32)
    badaT = PA.get(24, F32)
    gpreT = PA.get(8, F32)
    ssx = PA.get([NS, NT], F32)
    sdx = PA.get([NS, NT], F32)
    rsx = PA.get([NS, NT], F32)
    eps_t = PA.get(4, F32)
    eps_ap = eps_t[:, 0:1]
    eps4_ap = eps_t[:, 1:2]
    PHASE0 = PA.off
    LIMIT = ARENA_BYTES

    sp_q = "sp"

    def dma(eng, out, in_, r, w):
        return S.add(eng, lambda e, out=out, in_=in_: e.dma_start(out=out, in_=in_), r, w, dma=True)

    def mm(out, lhsT, rhs, start, stop, r, w):
        return S.add("pe", lambda e, out=out, lhsT=lhsT, rhs=rhs, start=start, stop=stop:
                     e.matmul(out, lhsT=lhsT, rhs=rhs, start=start, stop=stop), r, w)

    def tr(out, in_, ident, r, w):
        return S.add("pe", lambda e, out=out, in_=in_, ident=ident: e.transpose(out=out, in_=in_, identity=ident), r, w)

    def act(out, in_, func, r, w, bias=None, scale=None, accum_out=None):
        kw = {}
        if bias is not None:
            kw["bias"] = bias
        if scale is not None:
            kw["scale"] = scale
        if accum_out is not None:
            kw["accum_out"] = accum_out
        return S.add("act", lambda e, out=out, in_=in_, func=func, kw=kw: e.activation(out=out, in_=in_, func=func, **kw), r, w)

    def tt(eng, out, in0, in1, op, r, w):
        return S.add(eng, lambda e, out=out, in0=in0, in1=in1, op=op: e.tensor_tensor(out=out, in0=in0, in1=in1, op=op), r, w)

    def ts(eng, out, in0, s1, s2, op0, op1, r, w):
        if op1 is None:
            return S.add(eng, lambda e, out=out, in0=in0, s1=s1, op0=op0:
                         e.tensor_scalar(out=out, in0=in0, scalar1=s1, scalar2=None, op0=op0), r, w)
        return S.add(eng, lambda e, out=out, in0=in0, s1=s1, s2=s2, op0=op0, op1=op1:
                     e.tensor_scalar(out=out, in0=in0, scalar1=s1, scalar2=s2, op0=op0, op1=op1), r, w)

    def stt(eng, out, in0, scalar, in1, op0, op1, r, w):
        return S.add(eng, lambda e, out=out, in0=in0, scalar=scalar, in1=in1, op0=op0, op1=op1:
                     e.scalar_tensor_tensor(out=out, in0=in0, scalar=scalar, in1=in1, op0=op0, op1=op1), r, w)

    def cp(eng, out, in_, r, w):
        if eng == "act":
            return act(out, in_, AF.Copy, r, w)
        return S.add(eng, lambda e, out=out, in_=in_: e.tensor_copy(out=out, in_=in_), r, w)

    def recip(out, in_, r, w):
        return S.add("dve", lambda e, out=out, in_=in_: e.reciprocal(out=out, in_=in_), r, w)

    def memset(eng, ap, val, w):
        return S.add(eng, lambda e, ap=ap, val=val: e.memset(ap, val), (), w)

    def PS(i):
        return ("ps", i)

    for (dst, src, key) in ((ident_f, identf_d, "ident_f"), (ident_b, identb_d, "ident_b"), (tri_b, tri_d, "tri_b"),
                            (rm64, rm64_d, "rm64"), (rmmla, rmmla_d, "rmmla"), (invf, invf_d, "invf"),
                            (cb.rearrange("p a b -> p (a b)"), cb_d, "cb"), (gqT, gqT_d, "gqT"), (gkvT, gkvT_d, "gkvT"),
                            (badaT, badaT_d, "badaT"), (gpreT, gpreT_d, "gpreT")):
        dma("sp", dst, src, (), [key])
    memset("dve", ones_b, 1.0, ["ones_b"])
    memset("dve", half_b, 0.5, ["half_b"])
    memset("dve", ssx.rearrange("p a b -> p (a b)"), 0.0, ["ssx"])

    P0 = Alloc(PHASE0, LIMIT)
    c_sb = P0.get(8 * NS, F32)
    th0 = P0.get(8 * NS, F32)
    sc = P0.get([8, NS], F32)
    screp = P0.get([8 * NS, 128], F32)
    modT = P0.get([16, NS], F32)
    bmraw = P0.get(16, F32)
    wbuf = [P0.get([8, 512], F32) for _ in range(2)]
    badag = P0.get(DM, F32)
    gpost = P0.get(DM, F32)
    tmpg = P0.get(512, F32)

    dma("sp", c_sb, cT_d, (), ["c_sb"])
    dma("sp", bmraw, bmT_d, (), ["bmraw"])
    dma("sp", badag, badag_d, (), ["badag"])
    dma("sp", gpost, gpost_d, (), ["gpost"])
    ts("dve", bmh, bmraw, 0.5, None, ALU.mult, None, ["bmraw"], ["bmh"])
    act(th0, c_sb, AF.Tanh, ["c_sb"], ["th0"], scale=0.5)
    ts("dve", th0, th0, 0.5, 0.5, ALU.mult, ALU.add, ["th0"], ["th0"])
    tt("dve", sc.rearrange("p a b -> p (a b)"), th0, c_sb, ALU.mult, ["th0", "c_sb"], ["sc"])
    cp("dve", screp, sc.rearrange("p a b -> p (a b)").unsqueeze(2).to_broadcast([128, 8 * NS, 128]), ["sc"], ["screp"])
    wada_v = wada_d.rearrange("(k p) c -> p k c", p=128)
    for cc in range(6):
        wb = wbuf[cc % 2]
        dma("sp", wb, wada_v[:, :, cc * 512:(cc + 1) * 512], (), [("wbuf", cc % 2)])
        if cc < 4:
            for j in range(4):
                ct = cc * 4 + j
                for k in range(8):
                    mm(ps[0][:, ct * NS:(ct + 1) * NS], wb[:, k, j * 128:(j + 1) * 128], sc[:, k, :], k == 0, k == 7,
                       [("wbuf", cc % 2), "sc"], [PS(0)])
        else:
            for b in range(NS):
                for k in range(8):
                    mm(ps[1 + b][:, :], screp[:, k * NS + b, :], wb[:, k, :], k == 0, k == 7,
                       [("wbuf", cc % 2), "screp"], [PS(1 + b)])
                cols = slice((cc - 4) * 512, (cc - 3) * 512)
                tt("dve", tmpg, ps[1 + b][:, :], badag[:, cols], ALU.add, [PS(1 + b), "badag"], ["tmpg"])
                tt("dve", G2[:, b, cols], tmpg, gpost[:, cols], ALU.mult, ["tmpg", "gpost"], [("G2", b)])
        if cc == 3:
            tt("dve", modT, ps[0][:, 0:16 * NS].rearrange("p (a b) -> p a b", b=NS),
               badaT[:, 0:16].unsqueeze(2).to_broadcast([128, 16, NS]), ALU.add, [PS(0), "badaT"], ["modT"])
            for b in range(NS):
                stt("dve", A_sb[:, b, :], modT[:, 8:16, b], 1.0, gpreT, ALU.add, ALU.mult, ["modT", "gpreT"], [("AB", b)])
                cp("dve", B_sb[:, b, :], modT[:, 0:8, b], ["modT"], [("AB", b)])

    def hT_keys(c):
        return [("hT", t, e) for t in range(4 * c, 4 * c + 4) for e in (0, 1)]

    def phase_A(b):
        S.fence()
        PAa = Alloc(PHASE0, LIMIT)
        xt = [PAa.get(DM, F32) for _ in range(3)]
        xs = [PAa.get(DM, F32) for _ in range(2)]
        junk = PAa.get(DM, BF16)

        def load(t):
            dma("sp", xt[t % 3], x_d[b, t * 128:(t + 1) * 128, :], (), [("xt", t % 3)])

        def stage1(t):
            xi = t % 3
            si = t % 2
            act(junk, xt[xi], AF.Square, [("xt", xi), "ssx"], ["junk", ("ss", b, t)], accum_out=ssx[:, b, t:t + 1])
            act(sdx[:, b, t:t + 1], ssx[:, b, t:t + 1], AF.Sqrt, [("ss", b, t)], [("sd", b, t)], scale=1.0 / DM, bias=eps_ap)
            recip(rsx[:, b, t:t + 1], sdx[:, b, t:t + 1], [("sd", b, t)], [("rs", b, t)])
            ts("dve", xs[si], xt[xi], rsx[:, b, t:t + 1], None, ALU.mult, None, [("xt", xi), ("rs", b, t)], [("xs", si)])
            pb = 4 * (t % 2)
            for d in range(8):
                tr(ps[pb + d // 2][:, (d % 2) * 128:(d % 2 + 1) * 128], xs[si][:, d * 128:(d + 1) * 128], ident_f,
                   [("xs", si), "ident_f"], [PS(pb + d // 2)])

        def stage2(t):
            pb = 4 * (t % 2)
            for d in range(8):
                src = ps[pb + d // 2][:, (d % 2) * 128:(d % 2 + 1) * 128]
                dst = hT[:, d, t * 128:(t + 1) * 128]
                if d < 3:
                    act(dst, src, AF.Identity, [PS(pb + d // 2), ("AB", b)], [("hT", t, 0)],
                        scale=A_sb[:, b, d:d + 1], bias=B_sb[:, b, d:d + 1])
                else:
                    ts("dve", dst, src, A_sb[:, b, d:d + 1], B_sb[:, b, d:d + 1], ALU.mult, ALU.add,
                       [PS(pb + d // 2), ("AB", b)], [("hT", t, 1)])

        load(0)
        load(1)
        for t in range(NT + 1):
            if t + 2 < NT:
                load(t + 2)
            if t < NT:
                stage1(t)
            if t >= 1:
                stage2(t - 1)

    memset("dve", eps_t[:, 0:1], EPS, ["eps_t"])
    memset("dve", eps_t[:, 1:2], 4.0 * EPS, ["eps_t"])


    win_v = win_d.rearrange("(k p) c -> p k c", p=128)

    def build_tables(b, col, PB):
        tabc = PB.get(SEQ, BF16)
        tabs = PB.get(SEQ, BF16)
        mark = PB.off
        posi = [PB.get(512, I32) for _ in range(2)]
        pf = PB.get(512, F32)
        us = PB.get(512, F32)
        uc = PB.get(512, F32)
        ni = PB.get(512, I32)
        frs = PB.get(512, F32)
        frc = PB.get(512, F32)
        for c in range(NCH):
            pi_ = c % 2
            cs = slice(c * 512, (c + 1) * 512)
            dma("sp", posi[pi_], pos_d[b, cs].partition_broadcast(128), (), [("posi", pi_)])
            cp("dve", pf, posi[pi_], [("posi", pi_)], ["pf"])
            ts("dve", us, pf, invf[:, col:col + 1], None, ALU.mult, None, ["pf", "invf"], ["us"])
            ts("dve", uc, us, 0.25, None, ALU.add, None, ["us"], ["uc"])
            cp("dve", ni, us, ["us"], ["ni"])
            stt("dve", frs, ni, -1.0, us, ALU.mult, ALU.add, ["ni", "us"], ["frs"])
            act(tabs[:, cs], frs, AF.Sin, ["frs"], [("tab", c)], scale=TWO_PI)
            cp("dve", ni, uc, ["uc"], ["ni"])
            stt("dve", frc, ni, -1.0, uc, ALU.mult, ALU.add, ["ni", "uc"], ["frc"])
            act(tabc[:, cs], frc, AF.Sin, ["frc"], [("tab", c)], scale=TWO_PI)
        return tabc, tabs, mark

    gen_rr = [0]

    def run_attention(items, PT, GEN, scale):
        flat = []
        for ii, it in enumerate(items):
            for j in range(4 * it["c"] + 4):
                flat.append((ii, j))
        LA = 2

        def rng(it, j):
            c = it["c"]
            if j < 4 * c:
                return 0, 512
            return 128 * (j - 4 * c), 512

        if items and items[0].get("pre"):
            for grp in items[0]["pre"]:
                grp()
        first_gg = []
        acc = 0
        for it in items:
            first_gg.append(acc)
            acc += 4 * it["c"] + 4
        DEFER = 8
        pending = []
        for g in range(len(flat) + LA):
            if g < len(flat):
                ii, j = flat[g]
                it = items[ii]
                lo, hi = rng(it, j)
                sb = g % 3
                diag = j >= 4 * it["c"]
                c0 = it["c"] * 512
                mm(ps[sb][:, lo:hi], it["Kt"](j), it["Q"](c0 + lo, c0 + hi), True, not diag,
                   it["rK"](j) + it["rQ"], [PS(sb)])
                if diag:
                    mm(ps[sb][:, lo:lo + 128], ident_b, tri_b, False, True, ["ident_b", "tri_b"], [PS(sb)])
            gg = g - LA
            if gg >= 0:
                while pending and pending[0][0] <= gg:
                    pending.pop(0)[1]()
                ii, j = flat[gg]
                it = items[ii]
                lo, hi = rng(it, j)
                pb = gg % 4
                ob = 3 + ii % 2
                last = 4 * it["c"] + 3
                act(PT[pb][:, lo:hi], ps[gg % 3][:, lo:hi], AF.Exp, [PS(gg % 3)], [("PT", pb)], scale=scale)
                mm(ps[ob][0:65, lo:hi], it["V"](j), PT[pb][:, lo:hi], j == 0, j == last,
                   [("PT", pb)] + it["rV"](j), [PS(ob)])
                if j == last:
                    fin_b = it["fin"](ob, ii)
                    due = gg + DEFER
                    if ii + 2 < len(items):
                        due = min(due, first_gg[ii + 2])
                    pending.append((due, fin_b))
            if g < len(flat):
                ii, j = flat[g]
                nsteps = 4 * items[ii]["c"] + 4
                if ii + 1 < len(items) and items[ii + 1].get("pre"):
                    grps = items[ii + 1]["pre"]
                    if j < len(grps) and j < nsteps - 1:
                        grps[j]()
                    elif j == nsteps - 1:
                        for grp in grps[j:]:
                            grp()
        for _, fn in pending:
            fn()

    def finalize_head(ob, ii, sz2_ap, sz2_keys, rdrow3, u3, ogt, ogk, dst_ap, dst_key):
        k3 = ii % 3
        rdrow = rdrow3[k3]
        u = u3[k3]
        recip(rdrow[64:65, :], ps[ob][64:65, :], [PS(ob)], [("rdrow", k3)])
        tt("dve", u[0:64, :], ps[ob][0:64, :], sz2_ap, ALU.mult, [PS(ob)] + sz2_keys, [("u", k3)])

        def fin_b():
            mm(ps[ob][0:64, :], half_b[64:65, 0:64], rdrow[64:65, :], True, True, [("rdrow", k3), "half_b"], [PS(ob)])
            tt("dve", ogt[0:64, :], u[0:64, :], ps[ob][0:64, :], ALU.mult, [("u", k3), PS(ob)], [ogk])
            dma("sp", dst_ap, ogt[0:64, :], [ogk], [dst_key])
        return fin_b

    def phase_B1(b):
        S.fence()
        PB = Alloc(PHASE0, LIMIT)
        tabc, tabs, mark = build_tables(b, 1, PB)
        PB.off = mark
        qnT = PB.get([2, SEQ], BF16)
        kvnT = PB.get(SEQ, BF16)
        KPE = PB.get(SEQ, BF16)
        Qh = PB.get(SEQ, BF16)
        Kh = [PB.get(SEQ, BF16) for _ in range(2)]
        Vh = [PB.get([NT, 65], BF16) for _ in range(2)]
        w_zm = PB.get([8, 512], BF16)
        w_uq = PB.get([2, 768], BF16)
        w_ukv = PB.get(1024, BF16)
        t1 = PB.get(512, F32)
        t2 = PB.get(512, F32)
        mark2 = PB.off
        w_lat = PB.get([8, 416], BF16)
        sqb = [PB.get(512, BF16) for _ in range(3)]
        sdq = PB.get(512, F32)
        rq = PB.get(512, F32)
        sdkv = PB.get(512, F32)
        rkv = PB.get(512, F32)
        krb = PB.get(512, BF16)
        S.fence()

        dma("pool", w_lat, win_v[:, :, C_QLAT:C_ZMLA], (), ["w_lat"])
        dma("pool", w_zm, win_v[:, :, C_ZMLA:C_QMB], (), ["w_zm"])
        dma("pool", w_uq, wuq_d.rearrange("(k p) c -> p k c", p=128), (), ["w_uq"])
        dma("pool", w_ukv, wukv_d, (), ["w_ukv"])
        for i in range(2):
            memset("dve", Vh[i][:, :, 64:65], 1.0, [("V1", i)])

        for c in range(NCH):
            cs = slice(c * 512, (c + 1) * 512)
            hk = hT_keys(c)
            for j in range(2):
                for k in range(8):
                    mm(ps[j][:, :], w_lat[:, k, j * 128:(j + 1) * 128], hT[:, k, cs], k == 0, k == 7, ["w_lat"] + hk, [PS(j)])
            for k in range(8):
                mm(ps[2][:, :], w_lat[:, k, 256:384], hT[:, k, cs], k == 0, k == 7, ["w_lat"] + hk, [PS(2)])
            for k in range(8):
                mm(ps[3][0:32, :], w_lat[:, k, 384:416], hT[:, k, cs], k == 0, k == 7, ["w_lat"] + hk, [PS(3)])
            for j in range(3):
                act(sqb[j], ps[j][:, :], AF.Square, [PS(j)], [("sqb", j)])
            mm(ps[4][:, :], ones_b, sqb[0], True, False, ["ones_b", ("sqb", 0)], [PS(4)])
            mm(ps[4][:, :], ones_b, sqb[1], False, True, ["ones_b", ("sqb", 1)], [PS(4)])
            mm(ps[5][:, :], ones_b, sqb[2], True, True, ["ones_b", ("sqb", 2)], [PS(5)])
            act(sdq, ps[4][:, :], AF.Ln, [PS(4), "eps_t"], ["sdq"], scale=1.0 / 256, bias=eps_ap)
            act(sdkv, ps[5][:, :], AF.Ln, [PS(5), "eps_t"], ["sdkv"], scale=1.0 / 128, bias=eps_ap)
            act(rq, sdq, AF.Exp, ["sdq"], ["rq"], scale=-0.5)
            act(rkv, sdkv, AF.Exp, ["sdkv"], ["rkv"], scale=-0.5)
            for j in range(2):
                stt("dve", qnT[:, j, cs], ps[j][:, :], gqT[:, j:j + 1], rq, ALU.mult, ALU.mult, [PS(j), "gqT", "rq"], [("qnT", c)])
            stt("dve", kvnT[:, cs], ps[2][:, :], gkvT[:, 0:1], rkv, ALU.mult, ALU.mult, [PS(2), "gkvT", "rkv"], [("kvnT", c)])
            cp("act", krb[0:32, :], ps[3][0:32, :], [PS(3)], ["krb"])
            mm(ps[6][0:32, :], rmmla[0:32, 0:32], krb[0:32, :], True, True, ["rmmla", "krb"], [PS(6)])
            tt("dve", t1[0:32, :], ps[3][0:32, :], tabc[0:32, cs], ALU.mult, [PS(3), ("tab", c)], ["t1"])
            tt("dve", t2[0:32, :], ps[6][0:32, :], tabs[0:32, cs], ALU.mult, [PS(6), ("tab", c)], ["t2"])
            tt("dve", KPE[64:96, cs], t1[0:32, :], t2[0:32, :], ALU.add, ["t1", "t2"], [("KPE", c)])

        S.fence()
        PB.off = mark2
        PT = [PB.get(512, BF16) for _ in range(4)]
        th = PB.get(512, F32)
        SZh = PB.get(SEQ, BF16)
        rdrow = [PB.get(512, BF16) for _ in range(3)]
        u = [PB.get(512, F32) for _ in range(3)]
        ogt = [PB.get(512, BF16) for _ in range(2)]
        GEN = [5, 6, 7]

        def proj_chunk(h, c, kb):
            cs = slice(c * 512, (c + 1) * 512)
            G0, G1, G2, G3, G4 = [(gen_rr[0] + i) % 8 for i in range(5)]
            gen_rr[0] += 5
            for j in range(2):
                mm(ps[G0][0:96, :], w_uq[:, j, h * 96:(h + 1) * 96], qnT[:, j, cs], j == 0, j == 1, ["w_uq", ("qnT", c)], [PS(G0)])
            cp("dve", Qh[0:96, cs], ps[G0][0:96, :], [PS(G0)], [("Q", c, 0), ("Q", c, 1)])
            mm(ps[G1][0:64, :], w_ukv[:, h * 128:h * 128 + 64], kvnT[:, cs], True, True, ["w_ukv", ("kvnT", c)], [PS(G1)])
            cp("act", Kh[kb][0:64, cs], ps[G1][0:64, :], [PS(G1)], [("K", kb, c, 0)])
            cp("pool", Kh[kb][64:96, cs], KPE[64:96, cs], [("KPE", c)], [("K", kb, c, 1)])
            if h % 2 == 0:
                for k in range(8):
                    mm(ps[G2][:, :], w_zm[:, k, h * 64:(h + 2) * 64], hT[:, k, cs], k == 0, k == 7, ["w_zm"] + hT_keys(c), [PS(G2)])
                act(th, ps[G2][:, :], AF.Tanh, [PS(G2)], ["th"], scale=0.5)
                stt("dve", SZh[:, cs], th, 1.0, ps[G2][:, :], ALU.add, ALU.mult, ["th", PS(G2)], [("SZh", c)])
            mm(ps[G3][0:96, :], rmmla[64:96, 0:96], Qh[64:96, cs], True, True, ["rmmla", ("Q", c, 1)], [PS(G3)])
            tt("dve", t1[64:96, :], ps[G0][64:96, :], tabc[64:96, cs], ALU.mult, [PS(G0), ("tab", c)], ["t1"])
            tt("dve", t2[64:96, :], ps[G3][64:96, :], tabs[64:96, cs], ALU.mult, [PS(G3), ("tab", c)], ["t2"])
            tt("pool", Qh[64:96, cs], t1[64:96, :], t2[64:96, :], ALU.add, ["t1", "t2"], [("Q", c, 1)])
            for i in range(4):
                tsl = slice(c * 512 + i * 128, c * 512 + (i + 1) * 128)
                mm(ps[G4][:, i * 64:(i + 1) * 64], kvnT[:, tsl], w_ukv[:, h * 128 + 64:h * 128 + 128], True, True,
                   ["w_ukv", ("kvnT", c)], [PS(G4)])
            cp("act", Vh[kb][:, 4 * c:4 * c + 4, 0:64], ps[G4][:, 0:256].rearrange("p (i d) -> p i d", i=4), [PS(G4)], [("V", kb, c)])

        for h in range(8):
            kb = h % 2
            for c in range(NCH):
                proj_chunk(h, c, kb)
            items = []
            for c in range(NCH):
                def fin(ob, ii, h=h, c=c):
                    k = (h * NCH + c) % 2
                    r0 = (h % 2) * 64
                    return finalize_head(ob, ii, SZh[r0:r0 + 64, c * 512:(c + 1) * 512], [("SZh", c)], rdrow, u, ogt[k], ("ogt", k),
                                         ogscr_d[0, h // 2, (h % 2) * 64:(h % 2) * 64 + 64, c * 512:(c + 1) * 512], ("ogscr", 0, h // 2, c, h % 2))
                items.append(dict(
                    c=c, R=96,
                    Kt=lambda j, kb=kb: Kh[kb][0:96, j * 128:(j + 1) * 128],
                    Q=lambda lo, hi: Qh[0:96, lo:hi],
                    V=lambda j, kb=kb: Vh[kb][:, j, 0:65],
                    rK=lambda j, kb=kb: [("K", kb, j // 4, 0), ("K", kb, j // 4, 1)],
                    rQ=[("Q", c, 0), ("Q", c, 1)],
                    rV=lambda j, kb=kb: [("V", kb, j // 4), ("V1", kb)],
                    fin=fin, pre=None))
            run_attention(items, PT, GEN, 96 ** -0.5)


    def phase_B2(b):
        S.fence()
        PB = Alloc(PHASE0, LIMIT)
        tabc, tabs, mark = build_tables(b, 0, PB)
        PB.off = mark
        QA = [PB.get(SEQ, BF16) for _ in range(2)]
        KA = [PB.get(SEQ, BF16) for _ in range(2)]
        VA = [PB.get([NT, 65], BF16) for _ in range(2)]
        SZ = PB.get(SEQ, BF16)
        wq2 = [PB.get([8, 128], BF16) for _ in range(2)]
        wk2 = [PB.get([8, 128], BF16) for _ in range(2)]
        wv2 = [PB.get([8, 128], BF16) for _ in range(2)]
        wz2 = [PB.get([8, 128], BF16) for _ in range(2)]
        MB = PB.get([NT, 80], F32)
        gm = [PB.get([NT, 16], F32) for _ in range(2)]
        top8 = [PB.get([NT, 8], F32) for _ in range(2)]
        selb = PB.get([NT, 16], F32)
        ksum = [PB.get(16, F32) for _ in range(2)]
        kmh = [PB.get(16, BF16) for _ in range(2)]
        kml = [PB.get(16, BF16) for _ in range(2)]
        PT = [PB.get(512, BF16) for _ in range(4)]
        qpb = PB.get(512, BF16)
        t1 = PB.get(512, F32)
        t2 = PB.get(512, F32)
        th = PB.get(512, F32)
        rdrow = [PB.get(512, BF16) for _ in range(3)]
        u = [PB.get(512, F32) for _ in range(3)]
        ogt = [PB.get(512, BF16) for _ in range(2)]
        S.fence()
        GEN = [5, 6, 7]

        memset("dve", MB.rearrange("p a b -> p (a b)"), 0.0, ["MB"])
        for i in range(2):
            memset("dve", VA[i][:, :, 64:65], 1.0, [("V1", i)])
            dma("sp", KA[i][64:80, :], eind_d, (), [("KE", i)])

        def load_pair_w(p):
            i = p % 2
            dma("pool", wq2[i], win_v[:, :, C_QMB + p * 128:C_QMB + (p + 1) * 128], (), [("wq", i)])
            dma("pool", wk2[i], win_v[:, :, C_KMB + p * 128:C_KMB + (p + 1) * 128], (), [("wk", i)])
            dma("pool", wv2[i], win_v[:, :, C_VMB + p * 128:C_VMB + (p + 1) * 128], (), [("wv", i)])
            dma("pool", wz2[i], win_v[:, :, C_ZMB + p * 128:C_ZMB + (p + 1) * 128], (), [("wz", i)])

        load_pair_w(0)
        for p in range(4):
            if p + 1 < 4:
                load_pair_w(p + 1)
            wq, wk, wv, wz = wq2[p % 2], wk2[p % 2], wv2[p % 2], wz2[p % 2]
            wqk, wkk, wvk, wzk = ("wq", p % 2), ("wk", p % 2), ("wv", p % 2), ("wz", p % 2)
            for c in range(NCH):
                cs = slice(c * 512, (c + 1) * 512)
                hk = hT_keys(c)
                for (w_, wkey, dst, dkey) in ((wq, wqk, QA, "QAd"), (wk, wkk, KA, "KAd")):
                    g0 = gen_rr[0] % 8; g1 = (gen_rr[0] + 1) % 8
                    gen_rr[0] += 2
                    for k in range(8):
                        mm(ps[g0][:, :], w_[:, k, :], hT[:, k, cs], k == 0, k == 7, [wkey] + hk, [PS(g0)])
                    cp("act", qpb, ps[g0][:, :], [PS(g0)], ["qpb"])
                    mm(ps[g1][:, :], rm64, qpb, True, True, ["rm64", "qpb"], [PS(g1)])
                    tt("dve", t1, ps[g0][:, :], tabc[:, cs], ALU.mult, [PS(g0), ("tab", c)], ["t1"])
                    tt("dve", t2, ps[g1][:, :], tabs[:, cs], ALU.mult, [PS(g1), ("tab", c)], ["t2"])
                    tt("dve", dst[0][0:64, cs], t1[0:64, :], t2[0:64, :], ALU.add, ["t1", "t2"], [(dkey, 0, c)])
                    tt("dve", dst[1][0:64, cs], t1[64:128, :], t2[64:128, :], ALU.add, ["t1", "t2"], [(dkey, 1, c)])
                g0 = gen_rr[0] % 8; g1 = (gen_rr[0] + 1) % 8
                gen_rr[0] += 2
                for i in range(4):
                    tsl = slice(c * 512 + i * 128, c * 512 + (i + 1) * 128)
                    for k in range(8):
                        mm(ps[g0][:, i * 128:(i + 1) * 128], hT[:, k, tsl], wv[:, k, :], k == 0, k == 7, [wvk] + hk, [PS(g0)])
                pv = ps[g0][:, :].rearrange("p (i h d) -> p i h d", i=4, h=2)
                cp("act", VA[0][:, 4 * c:4 * c + 4, 0:64], pv[:, :, 0, :], [PS(g0)], [("V", 0, c)])
                cp("act", VA[1][:, 4 * c:4 * c + 4, 0:64], pv[:, :, 1, :], [PS(g0)], [("V", 1, c)])
                for k in range(8):
                    mm(ps[g1][:, :], wz[:, k, :], hT[:, k, cs], k == 0, k == 7, [wzk] + hk, [PS(g1)])
                act(th, ps[g1][:, :], AF.Tanh, [PS(g1)], ["th"], scale=0.5)
                stt("dve", SZ[:, cs], th, 1.0, ps[g1][:, :], ALU.add, ALU.mult, ["th", PS(g1)], [("SZ", c)])

            qkeys = [[("QAd", hh, c) for c in range(NCH)] for hh in range(2)]
            kkeys = [[("KAd", hh, c) for c in range(NCH)] for hh in range(2)]
            gbs = [gen_rr[0] % 8, (gen_rr[0] + 1) % 8]
            gen_rr[0] += 2
            for hh in range(2):
                S.add("dve", lambda e, hh=hh: e.tensor_reduce(out=ksum[hh][0:64, :], in_=KA[hh][0:64, :].rearrange("p (n k) -> p n k", n=16),
                                                             axis=AX.X, op=ALU.add), kkeys[hh], [("ksum", hh)])
            for hh in range(2):
                ts("dve", kmh[hh][0:64, :], ksum[hh][0:64, :], 1.0 / 256, None, ALU.mult, None, [("ksum", hh)], [("kmh", hh)])
                stt("dve", kml[hh][0:64, :], ksum[hh][0:64, :], 1.0 / 256, kmh[hh][0:64, :], ALU.mult, ALU.subtract,
                    [("ksum", hh), ("kmh", hh)], [("kml", hh)])
            for hh in range(2):
                gb = gbs[hh]
                for qt in range(NT):
                    mm(ps[gb][:, qt * 16:(qt + 1) * 16], QA[hh][0:64, qt * 128:(qt + 1) * 128], kmh[hh][0:64, :], True, False,
                       qkeys[hh] + [("kmh", hh)], [PS(gb)])
                    mm(ps[gb][:, qt * 16:(qt + 1) * 16], QA[hh][0:64, qt * 128:(qt + 1) * 128], kml[hh][0:64, :], False, True,
                       qkeys[hh] + [("kml", hh)], [PS(gb)])
            for hh in range(2):
                tt("dve", gm[hh].rearrange("p a b -> p (a b)"), ps[gbs[hh]][:, :], cb.rearrange("p a b -> p (a b)"), ALU.add,
                   [PS(gbs[hh]), "cb"], [("gm", hh)])
            for hh in range(2):
                for qt in range(NT):
                    S.add("dve", lambda e, qt=qt, hh=hh: e.max(out=top8[hh][:, qt, :], in_=gm[hh][:, qt, :]), [("gm", hh)], [("top8", hh, qt)])
            for hh in range(2):
                tt("dve", selb, gm[hh], top8[hh][:, :, 3:4].to_broadcast([128, NT, 16]), ALU.is_ge,
                   [("gm", hh)] + [("top8", hh, qt) for qt in range(NT)], ["selb"])
                ts("dve", MB[:, :, 64:80], selb, -1.0, -NEG, ALU.add, ALU.mult, ["selb", "MB"], ["MBd"])
                for grp in range(NCH):
                    g0 = gen_rr[0] % 8
                    gen_rr[0] += 1
                    for i in range(4):
                        qt = grp * 4 + i
                        tr(ps[g0][0:80, i * 128:(i + 1) * 128], MB[:, qt, :], ident_f, ["MBd", "ident_f"], [PS(g0)])
                    cp("act", QA[hh][64:80, grp * 512:(grp + 1) * 512], ps[g0][64:80, :], [PS(g0)], [("QAm", hh, grp)])

            items = []
            for hh in range(2):
                h = 2 * p + hh
                for c in range(NCH):
                    def fin(ob, ii, h=h, hh=hh, c=c):
                        k = (h * NCH + c) % 2
                        return finalize_head(ob, ii, SZ[hh * 64:hh * 64 + 64, c * 512:(c + 1) * 512], [("SZ", c)], rdrow, u, ogt[k], ("ogt", k),
                                      ogscr_d[1, h // 2, (h % 2) * 64:(h % 2) * 64 + 64, c * 512:(c + 1) * 512], ("ogscr", 1, h // 2, c, h % 2))
                    items.append(dict(
                        c=c, R=80,
                        Kt=lambda j, hh=hh: KA[hh][0:80, j * 128:(j + 1) * 128],
                        Q=lambda lo, hi, hh=hh: QA[hh][0:80, lo:hi],
                        V=lambda j, hh=hh: VA[hh][:, j, 0:65],
                        rK=lambda j, hh=hh: [("KAd", hh, j // 4), ("KE", hh)],
                        rQ=[("QAd", hh, c), ("QAm", hh, c)],
                        rV=lambda j, hh=hh: [("V", hh, j // 4), ("V1", hh)],
                        fin=fin, pre=None))
            run_attention(items, PT, GEN, 64 ** -0.5)


    def phase_C(b):
        S.fence()
        PB = Alloc(PHASE0, LIMIT)
        wm = PB.get([8, 2048], BF16)
        woa = PB.get([4, DM], BF16)
        wob = PB.get([4, DM], BF16)
        wout = PB.get([8, DM], BF16)
        oga = [PB.get([4, 512], BF16) for _ in range(2)]
        ogb = [PB.get([4, 512], BF16) for _ in range(2)]
        ta = [PB.get(512, F32) for _ in range(2)]
        tb = [PB.get(512, F32) for _ in range(2)]
        m1 = [PB.get(512, F32) for _ in range(2)]
        m2 = [PB.get(512, F32) for _ in range(2)]
        ymT = PB.get([8, 512], BF16)
        crr = [0]
        xt2 = [PB.get(DM, F32) for _ in range(2)]
        ot = [PB.get(DM, F32) for _ in range(2)]
        junk = PB.get(DM, BF16)
        ssy = PB.get(4, F32)
        S.fence()
        dma("pool", wm, win_v[:, :, C_MERGE:D_IN], (), ["wm"])
        dma("pool", woa, womla_d.rearrange("(k p) c -> p k c", p=128), (), ["woa"])
        dma("pool", wob, womb_d.rearrange("(k p) c -> p k c", p=128), (), ["wob"])
        dma("pool", wout, wout_d.rearrange("(k p) c -> p k c", p=128), (), ["wout"])
        def load_og(c):
            cs_ = slice(c * 512, (c + 1) * 512)
            oi_ = c % 2
            for pp in range(4):
                dma("sp", oga[oi_][:, pp, :], ogscr_d[0, pp, :, cs_], [("ogscr", 0, pp, c, 0), ("ogscr", 0, pp, c, 1)], [("oga", oi_)])
                dma("sp", ogb[oi_][:, pp, :], ogscr_d[1, pp, :, cs_], [("ogscr", 1, pp, c, 0), ("ogscr", 1, pp, c, 1)], [("ogb", oi_)])

        load_og(0)
        for c in range(NCH):
            cs = slice(c * 512, (c + 1) * 512)
            hk = hT_keys(c)
            oi = c % 2
            if c + 1 < NCH:
                load_og(c + 1)
            for d in range(8):
                ds_ = slice(d * 128, (d + 1) * 128)
                b0_, b1_, b2_, b3_ = [(crr[0] + i) % 8 for i in range(4)]
                crr[0] += 4
                for k in range(8):
                    mm(ps[b0_][:, :], wm[:, k, d * 128:(d + 1) * 128], hT[:, k, cs], k == 0, k == 7, ["wm"] + hk, [PS(b0_)])
                for k in range(8):
                    mm(ps[b1_][:, :], wm[:, k, DM + d * 128:DM + (d + 1) * 128], hT[:, k, cs], k == 0, k == 7, ["wm"] + hk, [PS(b1_)])
                for k in range(4):
                    mm(ps[b2_][:, :], woa[:, k, ds_], oga[oi][:, k, :], k == 0, k == 3, ["woa", ("oga", oi)], [PS(b2_)])
                for k in range(4):
                    mm(ps[b3_][:, :], wob[:, k, ds_], ogb[oi][:, k, :], k == 0, k == 3, ["wob", ("ogb", oi)], [PS(b3_)])
                di = d % 2
                act(ta[di], ps[b0_][:, :], AF.Tanh, [PS(b0_), "bmh"], [("ta", di)], scale=0.5, bias=bmh[:, d:d + 1])
                act(tb[di], ps[b1_][:, :], AF.Tanh, [PS(b1_), "bmh"], [("tb", di)], scale=0.5, bias=bmh[:, 8 + d:9 + d])
                stt("dve", m1[di], ta[di], 1.0, ps[b2_][:, :], ALU.add, ALU.mult, [("ta", di), PS(b2_)], [("m1", di)])
                stt("dve", m2[di], tb[di], 1.0, ps[b3_][:, :], ALU.add, ALU.mult, [("tb", di), PS(b3_)], [("m2", di)])
                tt("pool", ymT[:, d, :], m1[di], m2[di], ALU.add, [("m1", di), ("m2", di)], [("ymT", d)])
            for i in range(4):
                t = 4 * c + i
                xi = t % 2
                dma("sp", xt2[xi], x_d[b, t * 128:(t + 1) * 128, :], (), [("xt2", xi)])
                pb = crr[0] % 8
                crr[0] += 2
                if pb == 7:
                    pb = 0
                    crr[0] += 1
                for half in range(2):
                    for k in range(8):
                        mm(ps[pb + half][:, :], ymT[:, k, i * 128:(i + 1) * 128], wout[:, k, half * 512:(half + 1) * 512],
                           k == 0, k == 7, ["wout", ("ymT", k)], [PS(pb + half)])
                memset("dve", ssy[:, 0:2], 0.0, [("ssy", 0), ("ssy", 1), "ssyz"])
                for half in range(2):
                    act(junk[:, half * 512:(half + 1) * 512], ps[pb + half][:, :], AF.Square, [PS(pb + half), "ssyz"], ["junk", ("ssy", half)],
                        accum_out=ssy[:, half:half + 1])
                tt("dve", ssy[:, 2:3], ssy[:, 0:1], ssy[:, 1:2], ALU.add, [("ssy", 0), ("ssy", 1)], ["ssy2"])
                act(ssy[:, 3:4], ssy[:, 2:3], AF.Sqrt, ["ssy2", "eps_t"], ["ssy3"], scale=1.0 / DM, bias=eps4_ap)
                recip(ssy[:, 2:3], ssy[:, 3:4], ["ssy3"], ["ssy2"])
                for half in range(2):
                    hs = slice(half * 512, (half + 1) * 512)
                    stt("dve", ot[xi][:, hs], ps[pb + half][:, :], ssy[:, 2:3], G2[:, b, hs], ALU.mult, ALU.mult,
                        [PS(pb + half), "ssy2", ("G2", b)], [("ot", xi)])
                tt("pool", ot[xi], ot[xi], xt2[xi], ALU.add, [("ot", xi), ("xt2", xi)], [("ot", xi)])
                dma("pool", out_d[b, t * 128:(t + 1) * 128, :], ot[xi], [("ot", xi)], [("outst", b, t)])

    if debug == "0":
        d_A = dbg("A", [128, NS * 8]); d_B = dbg("B", [128, NS * 8]); d_G = dbg("G2", [128, NS * DM])
        dma("sp", d_A, A_sb.rearrange("p a b -> p (a b)"), [("AB", b) for b in range(NS)], [("dbg", 0)])
        dma("sp", d_B, B_sb.rearrange("p a b -> p (a b)"), [("AB", b) for b in range(NS)], [("dbg", 1)])
        dma("sp", d_G, G2.rearrange("p a b -> p (a b)"), [("G2", b) for b in range(NS)], [("dbg", 2)])
    for b in range(NS if debug != "0" else 0):
        phase_A(b)
        if debug == "A":
            d_h = dbg("hT%d" % b, [8, 128, SEQ], BF16)
            for d in range(8):
                dma("sp", d_h[d], hT[:, d, :], [k for c in range(NCH) for k in hT_keys(c)], [("dbg", b, d)])
            continue
        phase_B1(b)
        if debug == "B1":
            d_o = dbg("ogmla%d" % b, [4, 128, SEQ], BF16)
            S.fence()
            for pp in range(4):
                dma("sp", d_o[pp], ogscr_d[0, pp], (), [("dbg", b, pp)])
            continue
        if debug == "B2only":
            pass
        phase_B2(b)
        if debug == "B2":
            d_o = dbg("ogmb%d" % b, [4, 128, SEQ], BF16)
            S.fence()
            for pp in range(4):
                dma("sp", d_o[pp], ogscr_d[1, pp], (), [("dbg", b, pp)])
            continue
        phase_C(b)

    S.add("sp", None, r=[k for k in list(S.last_w.keys()) if isinstance(k, tuple) and k and k[0] in ("dbg", "outst")], w=())
    S.finalize()
    sems = {}
    for s, n in S.counts.items():
        nsem = 1 if s[0] == "dma" else (n - 1) // S.PER_SEM + 1
        if s[0] == "dma":
            assert n * 16 < 60000, (s, n)
        nm = "_".join(str(v) for v in s)
        sems[s] = [es.enter_context(nc.semaphore("sem_%s_%d" % (nm, k))) for k in range(nsem)]
    with nc.allow_low_precision(reason="bf16 matmul operands by design (fp32 PSUM accumulation)"), nc.Block() as block:
        S.emit(nc, block, sems)
    es.close()
    return nc, dbg_out


def _prep_inputs(inputs, NS, core):
    f32 = np.float32
    b0 = core * NS
    g = lambda k: np.asarray(inputs[k])
    m = {}
    m["x"] = np.ascontiguousarray(g("x")[b0:b0 + NS]).astype(f32, copy=False)
    m["pos"] = np.ascontiguousarray(g("positions")[b0:b0 + NS]).astype(np.int32, copy=False)
    c = g("c")[b0:b0 + NS].astype(f32)
    cT = c.reshape(NS, 8, 128).transpose(2, 1, 0)
    m["cT"] = np.ascontiguousarray(cT.reshape(128, 8 * NS))
    m["w_ada"] = np.ascontiguousarray(g("w_ada")[0], dtype=f32)
    bada = g("b_ada")[0].astype(f32)
    m["badaT"] = _T(bada, 24)
    m["badag_bc"] = np.ascontiguousarray(np.broadcast_to(bada[2048:3072][None, :], (128, DM)))
    m["gpreT"] = _T(g("g_pre")[0], 8)
    m["gpost_bc"] = np.ascontiguousarray(np.broadcast_to(g("g_post")[0].astype(f32)[None, :], (128, DM)))
    m["w_in"] = np.ascontiguousarray(g("w_in")[0], dtype=f32)
    m["gqT"] = _T(g("g_q_lat")[0], 2)
    m["gkvT"] = _T(g("g_kv_lat")[0], 1)
    m["w_uq"] = np.ascontiguousarray(g("w_uq")[0], dtype=f32)
    m["w_ukv"] = np.ascontiguousarray(g("w_ukv")[0], dtype=f32)
    m["w_o_mla"] = np.ascontiguousarray(g("w_o_mla")[0], dtype=f32)
    m["w_o_mb"] = np.ascontiguousarray(g("w_o_moba")[0], dtype=f32)
    m["bmT"] = _T(g("b_merge")[0], 16)
    m["w_out"] = np.ascontiguousarray(g("w_out")[0], dtype=f32)
    m.update(_consts())
    return m


def kernel(**inputs):
    NS = 2
    nc, _ = build(NS=NS)
    in_maps = [_prep_inputs(inputs, NS, core) for core in range(NCORES)]
    res = run_bass_kernel_spmd(nc, in_maps, core_ids=list(range(NCORES)))
    out = np.concatenate([np.asarray(r["out"]) for r in res.results], axis=0)
    return out.astype(np.float32, copy=False)
```

```python
import numpy as np
import ml_dtypes
import concourse.bass as bass
import concourse.mybir as mybir
from concourse.bass_utils import run_bass_kernel_spmd

F32 = mybir.dt.float32
BF16 = mybir.dt.bfloat16
I32 = mybir.dt.int32
ALU = mybir.AluOpType
AF = mybir.ActivationFunctionType
AX = mybir.AxisListType

NCORES = 8
SEQ = 4096
DM = 1024
D_IN = 5024
NCH = 8
NT = 32
EPS = 1e-6
NEG = -30000.0
TWO_PI = 6.283185307179586
PI = 3.141592653589793

C_QLAT, C_KVLAT, C_KROPE, C_ZMLA, C_QMB, C_KMB, C_VMB, C_ZMB, C_MERGE = 0, 256, 384, 416, 928, 1440, 1952, 2464, 2976


class Sched:
    ENGS = ("pe", "act", "dve", "pool", "sp")
    PER_SEM = 30000

    NDMASEM = 16

    def __init__(self):
        self.ops = []
        self.last_w = {}
        self.readers = {}
        self.dma_ops = {}
        self.last_of_stream = {}
        self.fence_deps = set()
        self.fence_pending = set()

    def add(self, eng, fn, r=(), w=(), dma=False):
        i = len(self.ops)
        deps = set()
        for res in r:
            j = self.last_w.get(res)
            if j is not None:
                deps.add(j)
            if isinstance(res, tuple) and res[0] == "ps":
                for j in self.readers.get(res, ()):
                    if self.ops[j]["eng"] != eng:
                        deps.add(j)
        for res in w:
            j = self.last_w.get(res)
            if j is not None:
                deps.add(j)
            for j in self.readers.get(res, ()):
                deps.add(j)
        for res in r:
            self.readers.setdefault(res, []).append(i)
        for res in w:
            self.last_w[res] = i
            self.readers[res] = []
        deps.discard(i)
        dman = None
        if dma:
            lst = self.dma_ops.setdefault(eng, [])
            dman = len(lst)
            if dman >= self.NDMASEM:
                deps.add(lst[dman - self.NDMASEM])
            lst.append(i)
        if eng in self.fence_pending:
            deps |= self.fence_deps
            self.fence_pending.discard(eng)
        self.ops.append(dict(eng=eng, fn=fn, deps=deps, dma=dma, dman=dman))
        self.last_of_stream[self.stream_of(i)] = i
        return i

    def fence(self):
        self.fence_deps = set(self.last_of_stream.values())
        self.fence_pending = set(self.ENGS)

    def stream_of(self, i):
        o = self.ops[i]
        return ("dma", o["eng"], o["dman"] % self.NDMASEM) if o["dma"] else ("eng", o["eng"])

    def finalize(self):
        waited = {}
        for i, o in enumerate(self.ops):
            best = {}
            for j in o["deps"]:
                s = self.stream_of(j)
                if s == ("eng", "pe") and o["eng"] == "pe" and not o["dma"]:
                    continue
                if j > best.get(s, -1):
                    best[s] = j
            waits = []
            for s, j in best.items():
                key = (o["eng"], s)
                if waited.get(key, -1) >= j:
                    continue
                waited[key] = j
                waits.append(j)
            o["waits"] = waits
        signal = set()
        for o in self.ops:
            signal.update(o["waits"])
        counts = {}
        for i, o in enumerate(self.ops):
            if o["dma"]:
                s = self.stream_of(i)
                n = o["dman"] // self.NDMASEM + 1
                counts[s] = n
                o["sig"] = (s, n)
            elif i in signal:
                s = self.stream_of(i)
                n = counts.get(s, 0) + 1
                counts[s] = n
                o["sig"] = (s, n)
            else:
                o["sig"] = None
        self.counts = counts

    def emit(self, nc, block_ctx, sems):
        def sem_for(s, n):
            if s[0] == "dma":
                return sems[s][0], n * 16
            k = (n - 1) // self.PER_SEM
            return sems[s][k], ((n - 1) % self.PER_SEM) + 1

        def run(engname, e):
            for o in self.ops:
                if o["eng"] != engname:
                    continue
                for j in o["waits"]:
                    s, n = self.ops[j]["sig"]
                    sh, val = sem_for(s, n)
                    e.wait_ge(sh, val)
                if o["fn"] is None:
                    continue
                ins = o["fn"](e)
                if o["sig"] is not None:
                    s, n = o["sig"]
                    if s[0] == "dma":
                        ins.then_inc(sems[s][0], 16)
                    else:
                        ins.then_inc(sems[s][(n - 1) // self.PER_SEM], 1)

        @block_ctx.tensor
        def _(e):
            run("pe", e)

        @block_ctx.scalar
        def _(e):
            run("act", e)

        @block_ctx.vector
        def _(e):
            run("dve", e)

        @block_ctx.gpsimd
        def _(e):
            run("pool", e)

        @block_ctx.sync
        def _(e):
            run("sp", e)


def _consts():
    bf = ml_dtypes.bfloat16
    c = {}
    c["ident_f"] = np.eye(128, dtype=np.float32)
    c["ident_b"] = np.eye(128, dtype=np.float32).astype(bf)
    k = np.arange(128)[:, None]
    q = np.arange(128)[None, :]
    c["tri_b"] = np.where(k <= q, 0.0, NEG).astype(np.float32).astype(bf)
    rm64 = np.zeros((128, 128), np.float32)
    for base in (0, 64):
        for m in range(64):
            if m < 32:
                rm64[base + m + 32, base + m] = -1.0
            else:
                rm64[base + m - 32, base + m] = 1.0
    c["rm64"] = rm64.astype(bf)
    rmm = np.zeros((128, 128), np.float32)
    for base in (0, 32, 64, 96):
        for m in range(32):
            if m < 16:
                rmm[base + m + 16, base + m] = -1.0
            else:
                rmm[base + m - 16, base + m] = 1.0
    c["rmmla"] = rmm.astype(bf)
    p = np.arange(128)
    invf = np.zeros((128, 2), np.float32)
    f0 = (np.float32(10000.0) ** (-((p % 64) % 32).astype(np.float32) / np.float32(32))).astype(np.float32)
    f1 = (np.float32(10000.0) ** (-((p % 32) % 16).astype(np.float32) / np.float32(16))).astype(np.float32)
    invf[:, 0] = (f0.astype(np.float64) / (2 * np.pi)).astype(np.float32)
    invf[:, 1] = (f1.astype(np.float64) / (2 * np.pi)).astype(np.float32)
    c["invf"] = invf
    cb = np.zeros((32, 16), np.float32)
    for qt in range(32):
        own = qt // 2
        for n in range(16):
            cb[qt, n] = 0.0 if n < own else (1e30 if n == own else -1e30)
    c["cb"] = np.ascontiguousarray(np.broadcast_to(cb.reshape(1, 512), (128, 512))).astype(np.float32)
    eind = np.zeros((16, SEQ), np.float32)
    for n in range(16):
        eind[n, n * 256:(n + 1) * 256] = 1.0
    c["eind"] = eind.astype(bf)
    return c


def _T(v, ntile):
    return np.ascontiguousarray(np.asarray(v, np.float32).reshape(ntile, 128).T)


def build(NS=2, debug=None):
    nc = bass.Bass("TRN2", target_bir_lowering=False)
    S = Sched()
    dbg_out = {}

    def din(name, shape, dt=F32):
        return nc.dram_tensor(name, list(shape), dt, kind="ExternalInput").ap()

    x_d = din("x", [NS, SEQ, DM])
    pos_d = din("pos", [NS, SEQ], I32)
    cT_d = din("cT", [128, 8 * NS])
    wada_d = din("w_ada", [DM, 3 * DM])
    badaT_d = din("badaT", [128, 24])
    badag_d = din("badag_bc", [128, DM])
    gpreT_d = din("gpreT", [128, 8])
    gpost_d = din("gpost_bc", [128, DM])
    win_d = din("w_in", [DM, D_IN])
    gqT_d = din("gqT", [128, 2])
    gkvT_d = din("gkvT", [128, 1])
    wuq_d = din("w_uq", [256, 768])
    wukv_d = din("w_ukv", [128, 1024])
    womla_d = din("w_o_mla", [512, DM])
    womb_d = din("w_o_mb", [512, DM])
    bmT_d = din("bmT", [128, 16])
    wout_d = din("w_out", [DM, DM])
    identf_d = din("ident_f", [128, 128])
    identb_d = din("ident_b", [128, 128], BF16)
    tri_d = din("tri_b", [128, 128], BF16)
    rm64_d = din("rm64", [128, 128], BF16)
    rmmla_d = din("rmmla", [128, 128], BF16)
    invf_d = din("invf", [128, 2])
    cb_d = din("cb", [128, 512])
    eind_d = din("eind", [16, SEQ], BF16)
    out_d = nc.dram_tensor("out", [NS, SEQ, DM], F32, kind="ExternalOutput").ap()
    ogscr_d = nc.dram_tensor("og_scr", [2, 4, 128, SEQ], BF16, kind="Internal").ap()

    def dbg(name, shape, dt=F32):
        t = nc.dram_tensor("dbg_" + name, list(shape), dt, kind="ExternalOutput").ap()
        dbg_out[name] = t
        return t

    ARENA_BYTES = 210000
    from contextlib import ExitStack
    es = ExitStack()
    arena = es.enter_context(nc.sbuf_tensor("arena", [128, ARENA_BYTES // 2], BF16))
    ps = [es.enter_context(nc.psum_tensor("ps%d" % i, [128, 512], F32)) for i in range(8)]

    def carve(off, shape, dt):
        esz = 4 if dt in (F32, I32) else 2
        n = int(np.prod(shape))
        assert off % 4 == 0
        a = arena[:, off // 2: off // 2 + n * esz // 2]
        if dt != BF16:
            a = a.bitcast(dt)
        if len(shape) == 2:
            a = a.rearrange("p (a b) -> p a b", a=shape[0])
        elif len(shape) == 3:
            a = a.rearrange("p (a b c) -> p a b c", a=shape[0], b=shape[1])
        return a, off + n * esz

    class Alloc:
        def __init__(self, start, limit):
            self.off = start
            self.limit = limit

        def get(self, shape, dt):
            if isinstance(shape, int):
                shape = [shape]
            a, self.off = carve(self.off, shape, dt)
            self.off = (self.off + 31) // 32 * 32
            assert self.off <= self.limit, (self.off, self.limit)
            return a

    PA = Alloc(0, 82000)
    hT = PA.get([8, SEQ], BF16)
    ident_f = PA.get(128, F32)
    ident_b = PA.get(128, BF16)
    tri_b = PA.get(128, BF16)
    rm64 = PA.get(128, BF16)
    rmmla = PA.get(128, BF16)
    ones_b = PA.get(128, BF16)
    half_b = PA.get(128, BF16)
    cb = PA.get([32, 16], F32)
    invf = PA.get(2, F32)
    A_sb = PA.get([NS, 8], F32)
    B_sb = PA.get([NS, 8], F32)
    G2 = PA.get([NS, DM], F32)
    gqT = PA.get(2, F32)
    gkvT = PA.get(1, F32)
    bmh = PA.get(16, F32)
    badaT = PA.get(24, F32)
    gpreT = PA.get(8, F32)
    ssx = PA.get([NS, NT], F32)
    sdx = PA.get([NS, NT], F32)
    rsx = PA.get([NS, NT], F32)
    eps_t = PA.get(4, F32)
    eps_ap = eps_t[:, 0:1]
    eps4_ap = eps_t[:, 1:2]
    PHASE0 = PA.off
    LIMIT = ARENA_BYTES

    sp_q = "sp"

    def dma(eng, out, in_, r, w):
        return S.add(eng, lambda e, out=out, in_=in_: e.dma_start(out=out, in_=in_), r, w, dma=True)

    def mm(out, lhsT, rhs, start, stop, r, w):
        return S.add("pe", lambda e, out=out, lhsT=lhsT, rhs=rhs, start=start, stop=stop:
                     e.matmul(out, lhsT=lhsT, rhs=rhs, start=start, stop=stop), r, w)

    def tr(out, in_, ident, r, w):
        return S.add("pe", lambda e, out=out, in_=in_, ident=ident: e.transpose(out=out, in_=in_, identity=ident), r, w)

    def act(out, in_, func, r, w, bias=None, scale=None, accum_out=None):
        kw = {}
        if bias is not None:
            kw["bias"] = bias
        if scale is not None:
            kw["scale"] = scale
        if accum_out is not None:
            kw["accum_out"] = accum_out
        return S.add("act", lambda e, out=out, in_=in_, func=func, kw=kw: e.activation(out=out, in_=in_, func=func, **kw), r, w)

    def tt(eng, out, in0, in1, op, r, w):
        return S.add(eng, lambda e, out=out, in0=in0, in1=in1, op=op: e.tensor_tensor(out=out, in0=in0, in1=in1, op=op), r, w)

    def ts(eng, out, in0, s1, s2, op0, op1, r, w):
        if op1 is None:
            return S.add(eng, lambda e, out=out, in0=in0, s1=s1, op0=op0:
                         e.tensor_scalar(out=out, in0=in0, scalar1=s1, scalar2=None, op0=op0), r, w)
        return S.add(eng, lambda e, out=out, in0=in0, s1=s1, s2=s2, op0=op0, op1=op1:
                     e.tensor_scalar(out=out, in0=in0, scalar1=s1, scalar2=s2, op0=op0, op1=op1), r, w)

    def stt(eng, out, in0, scalar, in1, op0, op1, r, w):
        return S.add(eng, lambda e, out=out, in0=in0, scalar=scalar, in1=in1, op0=op0, op1=op1:
                     e.scalar_tensor_tensor(out=out, in0=in0, scalar=scalar, in1=in1, op0=op0, op1=op1), r, w)

    def cp(eng, out, in_, r, w):
        if eng == "act":
            return act(out, in_, AF.Copy, r, w)
        return S.add(eng, lambda e, out=out, in_=in_: e.tensor_copy(out=out, in_=in_), r, w)

    def recip(out, in_, r, w):
        return S.add("dve", lambda e, out=out, in_=in_: e.reciprocal(out=out, in_=in_), r, w)

    def memset(eng, ap, val, w):
        return S.add(eng, lambda e, ap=ap, val=val: e.memset(ap, val), (), w)

    def PS(i):
        return ("ps", i)

    for (dst, src, key) in ((ident_f, identf_d, "ident_f"), (ident_b, identb_d, "ident_b"), (tri_b, tri_d, "tri_b"),
                            (rm64, rm64_d, "rm64"), (rmmla, rmmla_d, "rmmla"), (invf, invf_d, "invf"),
                            (cb.rearrange("p a b -> p (a b)"), cb_d, "cb"), (gqT, gqT_d, "gqT"), (gkvT, gkvT_d, "gkvT"),
                            (badaT, badaT_d, "badaT"), (gpreT, gpreT_d, "gpreT")):
        dma("sp", dst, src, (), [key])
    memset("dve", ones_b, 1.0, ["ones_b"])
    memset("dve", half_b, 0.5, ["half_b"])
    memset("dve", ssx.rearrange("p a b -> p (a b)"), 0.0, ["ssx"])

    P0 = Alloc(PHASE0, LIMIT)
    c_sb = P0.get(8 * NS, F32)
    th0 = P0.get(8 * NS, F32)
    sc = P0.get([8, NS], F32)
    screp = P0.get([8 * NS, 128], F32)
    modT = P0.get([16, NS], F32)
    bmraw = P0.get(16, F32)
    wbuf = [P0.get([8, 512], F32) for _ in range(2)]
    badag = P0.get(DM, F32)
    gpost = P0.get(DM, F32)
    tmpg = P0.get(512, F32)

    dma("sp", c_sb, cT_d, (), ["c_sb"])
    dma("sp", bmraw, bmT_d, (), ["bmraw"])
    dma("sp", badag, badag_d, (), ["badag"])
    dma("sp", gpost, gpost_d, (), ["gpost"])
    ts("dve", bmh, bmraw, 0.5, None, ALU.mult, None, ["bmraw"], ["bmh"])
    act(th0, c_sb, AF.Tanh, ["c_sb"], ["th0"], scale=0.5)
    ts("dve", th0, th0, 0.5, 0.5, ALU.mult, ALU.add, ["th0"], ["th0"])
    tt("dve", sc.rearrange("p a b -> p (a b)"), th0, c_sb, ALU.mult, ["th0", "c_sb"], ["sc"])
    cp("dve", screp, sc.rearrange("p a b -> p (a b)").unsqueeze(2).to_broadcast([128, 8 * NS, 128]), ["sc"], ["screp"])
    wada_v = wada_d.rearrange("(k p) c -> p k c", p=128)
    for cc in range(6):
        wb = wbuf[cc % 2]
        dma("sp", wb, wada_v[:, :, cc * 512:(cc + 1) * 512], (), [("wbuf", cc % 2)])
        if cc < 4:
            for j in range(4):
                ct = cc * 4 + j
                for k in range(8):
                    mm(ps[0][:, ct * NS:(ct + 1) * NS], wb[:, k, j * 128:(j + 1) * 128], sc[:, k, :], k == 0, k == 7,
                       [("wbuf", cc % 2), "sc"], [PS(0)])
        else:
            for b in range(NS):
                for k in range(8):
                    mm(ps[1 + b][:, :], screp[:, k * NS + b, :], wb[:, k, :], k == 0, k == 7,
                       [("wbuf", cc % 2), "screp"], [PS(1 + b)])
                cols = slice((cc - 4) * 512, (cc - 3) * 512)
                tt("dve", tmpg, ps[1 + b][:, :], badag[:, cols], ALU.add, [PS(1 + b), "badag"], ["tmpg"])
                tt("dve", G2[:, b, cols], tmpg, gpost[:, cols], ALU.mult, ["tmpg", "gpost"], [("G2", b)])
        if cc == 3:
            tt("dve", modT, ps[0][:, 0:16 * NS].rearrange("p (a b) -> p a b", b=NS),
               badaT[:, 0:16].unsqueeze(2).to_broadcast([128, 16, NS]), ALU.add, [PS(0), "badaT"], ["modT"])
            for b in range(NS):
                stt("dve", A_sb[:, b, :], modT[:, 8:16, b], 1.0, gpreT, ALU.add, ALU.mult, ["modT", "gpreT"], [("AB", b)])
                cp("dve", B_sb[:, b, :], modT[:, 0:8, b], ["modT"], [("AB", b)])

    def hT_keys(c):
        return [("hT", t, e) for t in range(4 * c, 4 * c + 4) for e in (0, 1)]

    def phase_A(b):
        S.fence()
        PAa = Alloc(PHASE0, LIMIT)
        xt = [PAa.get(DM, F32) for _ in range(3)]
        xs = [PAa.get(DM, F32) for _ in range(2)]
        junk = PAa.get(DM, BF16)

        def load(t):
            dma("sp", xt[t % 3], x_d[b, t * 128:(t + 1) * 128, :], (), [("xt", t % 3)])

        def stage1(t):
            xi = t % 3
            si = t % 2
            act(junk, xt[xi], AF.Square, [("xt", xi), "ssx"], ["junk", ("ss", b, t)], accum_out=ssx[:, b, t:t + 1])
            act(sdx[:, b, t:t + 1], ssx[:, b, t:t + 1], AF.Sqrt, [("ss", b, t)], [("sd", b, t)], scale=1.0 / DM, bias=eps_ap)
            recip(rsx[:, b, t:t + 1], sdx[:, b, t:t + 1], [("sd", b, t)], [("rs", b, t)])
            ts("dve", xs[si], xt[xi], rsx[:, b, t:t + 1], None, ALU.mult, None, [("xt", xi), ("rs", b, t)], [("xs", si)])
            pb = 4 * (t % 2)
            for d in range(8):
                tr(ps[pb + d // 2][:, (d % 2) * 128:(d % 2 + 1) * 128], xs[si][:, d * 128:(d + 1) * 128], ident_f,
                   [("xs", si), "ident_f"], [PS(pb + d // 2)])

        def stage2(t):
            pb = 4 * (t % 2)
            for d in range(8):
                src = ps[pb + d // 2][:, (d % 2) * 128:(d % 2 + 1) * 128]
                dst = hT[:, d, t * 128:(t + 1) * 128]
                if d < 3:
                    act(dst, src, AF.Identity, [PS(pb + d // 2), ("AB", b)], [("hT", t, 0)],
                        scale=A_sb[:, b, d:d + 1], bias=B_sb[:, b, d:d + 1])
                else:
                    ts("dve", dst, src, A_sb[:, b, d:d + 1], B_sb[:, b, d:d + 1], ALU.mult, ALU.add,
                       [PS(pb + d // 2), ("AB", b)], [("hT", t, 1)])

        load(0)
        load(1)
        for t in range(NT + 1):
            if t + 2 < NT:
                load(t + 2)
            if t < NT:
                stage1(t)
            if t >= 1:
                stage2(t - 1)

    memset("dve", eps_t[:, 0:1], EPS, ["eps_t"])
    memset("dve", eps_t[:, 1:2], 4.0 * EPS, ["eps_t"])


    win_v = win_d.rearrange("(k p) c -> p k c", p=128)

    def build_tables(b, col, PB):
        tabc = PB.get(SEQ, BF16)
        tabs = PB.get(SEQ, BF16)
        mark = PB.off
        posi = [PB.get(512, I32) for _ in range(2)]
        pf = PB.get(512, F32)
        us = PB.get(512, F32)
        uc = PB.get(512, F32)
        ni = PB.get(512, I32)
        frs = PB.get(512, F32)
        frc = PB.get(512, F32)
        for c in range(NCH):
            pi_ = c % 2
            cs = slice(c * 512, (c + 1) * 512)
            dma("sp", posi[pi_], pos_d[b, cs].partition_broadcast(128), (), [("posi", pi_)])
            cp("dve", pf, posi[pi_], [("posi", pi_)], ["pf"])
            ts("dve", us, pf, invf[:, col:col + 1], None, ALU.mult, None, ["pf", "invf"], ["us"])
            ts("dve", uc, us, 0.25, None, ALU.add, None, ["us"], ["uc"])
            cp("dve", ni, us, ["us"], ["ni"])
            stt("dve", frs, ni, -1.0, us, ALU.mult, ALU.add, ["ni", "us"], ["frs"])
            act(tabs[:, cs], frs, AF.Sin, ["frs"], [("tab", c)], scale=TWO_PI)
            cp("dve", ni, uc, ["uc"], ["ni"])
            stt("dve", frc, ni, -1.0, uc, ALU.mult, ALU.add, ["ni", "uc"], ["frc"])
            act(tabc[:, cs], frc, AF.Sin, ["frc"], [("tab", c)], scale=TWO_PI)
        return tabc, tabs, mark

    gen_rr = [0]

    def run_attention(items, PT, GEN, scale):
        flat = []
        for ii, it in enumerate(items):
            for j in range(4 * it["c"] + 4):
                flat.append((ii, j))
        LA = 2

        def rng(it, j):
            c = it["c"]
            if j < 4 * c:
                return 0, 512
            return 128 * (j - 4 * c), 512

        if items and items[0].get("pre"):
            for grp in items[0]["pre"]:
                grp()
        first_gg = []
        acc = 0
        for it in items:
            first_gg.append(acc)
            acc += 4 * it["c"] + 4
        DEFER = 8
        pending = []
        for g in range(len(flat) + LA):
            if g < len(flat):
                ii, j = flat[g]
                it = items[ii]
                lo, hi = rng(it, j)
                sb = g % 3
                diag = j >= 4 * it["c"]
                c0 = it["c"] * 512
                mm(ps[sb][:, lo:hi], it["Kt"](j), it["Q"](c0 + lo, c0 + hi), True, not diag,
                   it["rK"](j) + it["rQ"], [PS(sb)])
                if diag:
                    mm(ps[sb][:, lo:lo + 128], ident_b, tri_b, False, True, ["ident_b", "tri_b"], [PS(sb)])
            gg = g - LA
            if gg >= 0:
                while pending and pending[0][0] <= gg:
                    pending.pop(0)[1]()
                ii, j = flat[gg]
                it = items[ii]
                lo, hi = rng(it, j)
                pb = gg % 4
                ob = 3 + ii % 2
                last = 4 * it["c"] + 3
                act(PT[pb][:, lo:hi], ps[gg % 3][:, lo:hi], AF.Exp, [PS(gg % 3)], [("PT", pb)], scale=scale)
                mm(ps[ob][0:65, lo:hi], it["V"](j), PT[pb][:, lo:hi], j == 0, j == last,
                   [("PT", pb)] + it["rV"](j), [PS(ob)])
                if j == last:
                    fin_b = it["fin"](ob, ii)
                    due = gg + DEFER
                    if ii + 2 < len(items):
                        due = min(due, first_gg[ii + 2])
                    pending.append((due, fin_b))
            if g < len(flat):
                ii, j = flat[g]
                nsteps = 4 * items[ii]["c"] + 4
                if ii + 1 < len(items) and items[ii + 1].get("pre"):
                    grps = items[ii + 1]["pre"]
                    if j < len(grps) and j < nsteps - 1:
                        grps[j]()
                    elif j == nsteps - 1:
                        for grp in grps[j:]:
                            grp()
        for _, fn in pending:
            fn()

    def finalize_head(ob, ii, sz2_ap, sz2_keys, rdrow3, u3, ogt, ogk, dst_ap, dst_key):
        k3 = ii % 3
        rdrow = rdrow3[k3]
        u = u3[k3]
        recip(rdrow[64:65, :], ps[ob][64:65, :], [PS(ob)], [("rdrow", k3)])
        tt("dve", u[0:64, :], ps[ob][0:64, :], sz2_ap, ALU.mult, [PS(ob)] + sz2_keys, [("u", k3)])

        def fin_b():
            mm(ps[ob][0:64, :], half_b[64:65, 0:64], rdrow[64:65, :], True, True, [("rdrow", k3), "half_b"], [PS(ob)])
            tt("dve", ogt[0:64, :], u[0:64, :], ps[ob][0:64, :], ALU.mult, [("u", k3), PS(ob)], [ogk])
            dma("sp", dst_ap, ogt[0:64, :], [ogk], [dst_key])
        return fin_b

    def phase_B1(b):
        S.fence()
        PB = Alloc(PHASE0, LIMIT)
        tabc, tabs, mark = build_tables(b, 1, PB)
        PB.off = mark
        qnT = PB.get([2, SEQ], BF16)
        kvnT = PB.get(SEQ, BF16)
        KPE = PB.get(SEQ, BF16)
        Qh = PB.get(SEQ, BF16)
        Kh = [PB.get(SEQ, BF16) for _ in range(2)]
        Vh = [PB.get([NT, 65], BF16) for _ in range(2)]
        w_zm = PB.get([8, 512], BF16)
        w_uq = PB.get([2, 768], BF16)
        w_ukv = PB.get(1024, BF16)
        t1 = PB.get(512, F32)
        t2 = PB.get(512, F32)
        mark2 = PB.off
        w_lat = PB.get([8, 416], BF16)
        sqb = [PB.get(512, BF16) for _ in range(3)]
        sdq = PB.get(512, F32)
        rq = PB.get(512, F32)
        sdkv = PB.get(512, F32)
        rkv = PB.get(512, F32)
        krb = PB.get(512, BF16)
        lraw = [PB.get(512, F32) for _ in range(3)]
        S.fence()

        dma("pool", w_lat, win_v[:, :, C_QLAT:C_ZMLA], (), ["w_lat"])
        dma("pool", w_zm, win_v[:, :, C_ZMLA:C_QMB], (), ["w_zm"])
        dma("pool", w_uq, wuq_d.rearrange("(k p) c -> p k c", p=128), (), ["w_uq"])
        dma("pool", w_ukv, wukv_d, (), ["w_ukv"])
        for i in range(2):
            memset("dve", Vh[i][:, :, 64:65], 1.0, [("V1", i)])

        for c in range(NCH):
            cs = slice(c * 512, (c + 1) * 512)
            hk = hT_keys(c)
            for j in range(2):
                for k in range(8):
                    mm(ps[j][:, :], w_lat[:, k, j * 128:(j + 1) * 128], hT[:, k, cs], k == 0, k == 7, ["w_lat"] + hk, [PS(j)])
            for k in range(8):
                mm(ps[2][:, :], w_lat[:, k, 256:384], hT[:, k, cs], k == 0, k == 7, ["w_lat"] + hk, [PS(2)])
            for k in range(8):
                mm(ps[3][0:32, :], w_lat[:, k, 384:416], hT[:, k, cs], k == 0, k == 7, ["w_lat"] + hk, [PS(3)])
            for j in range(3):
                act(sqb[j], ps[j][:, :], AF.Square, [PS(j)], [("sqb", j)])
                cp("dve", lraw[j], ps[j][:, :], [PS(j)], [("lraw", j)])
            mm(ps[4][:, :], ones_b, sqb[0], True, False, ["ones_b", ("sqb", 0)], [PS(4)])
            mm(ps[4][:, :], ones_b, sqb[1], False, True, ["ones_b", ("sqb", 1)], [PS(4)])
            mm(ps[5][:, :], ones_b, sqb[2], True, True, ["ones_b", ("sqb", 2)], [PS(5)])
            act(sdq, ps[4][:, :], AF.Ln, [PS(4), "eps_t"], ["sdq"], scale=1.0 / 256, bias=eps_ap)
            act(sdkv, ps[5][:, :], AF.Ln, [PS(5), "eps_t"], ["sdkv"], scale=1.0 / 128, bias=eps_ap)
            act(rq, sdq, AF.Exp, ["sdq"], ["rq"], scale=-0.5)
            act(rkv, sdkv, AF.Exp, ["sdkv"], ["rkv"], scale=-0.5)
            for j in range(2):
                stt("dve", qnT[:, j, cs], lraw[j], gqT[:, j:j + 1], rq, ALU.mult, ALU.mult, [("lraw", j), "gqT", "rq"], [("qnT", c)])
            stt("dve", kvnT[:, cs], lraw[2], gkvT[:, 0:1], rkv, ALU.mult, ALU.mult, [("lraw", 2), "gkvT", "rkv"], [("kvnT", c)])
            cp("act", krb[0:32, :], ps[3][0:32, :], [PS(3)], ["krb"])
            mm(ps[6][0:32, :], rmmla[0:32, 0:32], krb[0:32, :], True, True, ["rmmla", "krb"], [PS(6)])
            tt("dve", t1[0:32, :], ps[3][0:32, :], tabc[0:32, cs], ALU.mult, [PS(3), ("tab", c)], ["t1"])
            tt("dve", t2[0:32, :], ps[6][0:32, :], tabs[0:32, cs], ALU.mult, [PS(6), ("tab", c)], ["t2"])
            tt("dve", KPE[64:96, cs], t1[0:32, :], t2[0:32, :], ALU.add, ["t1", "t2"], [("KPE", c)])

        S.fence()
        PB.off = mark2
        PT = [PB.get(512, BF16) for _ in range(4)]
        th = PB.get(512, F32)
        SZh = PB.get(SEQ, BF16)
        rdrow = [PB.get(512, BF16) for _ in range(3)]
        u = [PB.get(512, F32) for _ in range(3)]
        ogt = [PB.get(512, BF16) for _ in range(2)]
        GEN = [5, 6, 7]

        def proj_chunk(h, c, kb):
            cs = slice(c * 512, (c + 1) * 512)
            G0, G1, G2, G3, G4 = [(gen_rr[0] + i) % 8 for i in range(5)]
            gen_rr[0] += 5
            for j in range(2):
                mm(ps[G0][0:96, :], w_uq[:, j, h * 96:(h + 1) * 96], qnT[:, j, cs], j == 0, j == 1, ["w_uq", ("qnT", c)], [PS(G0)])
            cp("dve", Qh[0:96, cs], ps[G0][0:96, :], [PS(G0)], [("Q", c, 0), ("Q", c, 1)])
            mm(ps[G1][0:64, :], w_ukv[:, h * 128:h * 128 + 64], kvnT[:, cs], True, True, ["w_ukv", ("kvnT", c)], [PS(G1)])
            cp("act", Kh[kb][0:64, cs], ps[G1][0:64, :], [PS(G1)], [("K", kb, c, 0)])
            cp("pool", Kh[kb][64:96, cs], KPE[64:96, cs], [("KPE", c)], [("K", kb, c, 1)])
            if h % 2 == 0:
                for k in range(8):
                    mm(ps[G2][:, :], w_zm[:, k, h * 64:(h + 2) * 64], hT[:, k, cs], k == 0, k == 7, ["w_zm"] + hT_keys(c), [PS(G2)])
                act(th, ps[G2][:, :], AF.Tanh, [PS(G2)], ["th"], scale=0.5)
                stt("dve", SZh[:, cs], th, 1.0, ps[G2][:, :], ALU.add, ALU.mult, ["th", PS(G2)], [("SZh", c)])
            mm(ps[G3][0:96, :], rmmla[64:96, 0:96], Qh[64:96, cs], True, True, ["rmmla", ("Q", c, 1)], [PS(G3)])
            tt("dve", t1[64:96, :], ps[G0][64:96, :], tabc[64:96, cs], ALU.mult, [PS(G0), ("tab", c)], ["t1"])
            tt("dve", t2[64:96, :], ps[G3][64:96, :], tabs[64:96, cs], ALU.mult, [PS(G3), ("tab", c)], ["t2"])
            tt("pool", Qh[64:96, cs], t1[64:96, :], t2[64:96, :], ALU.add, ["t1", "t2"], [("Q", c, 1)])
            for i in range(4):
                tsl = slice(c * 512 + i * 128, c * 512 + (i + 1) * 128)
                mm(ps[G4][:, i * 64:(i + 1) * 64], kvnT[:, tsl], w_ukv[:, h * 128 + 64:h * 128 + 128], True, True,
                   ["w_ukv", ("kvnT", c)], [PS(G4)])
            cp("act", Vh[kb][:, 4 * c:4 * c + 4, 0:64], ps[G4][:, 0:256].rearrange("p (i d) -> p i d", i=4), [PS(G4)], [("V", kb, c)])

        for h in range(8):
            kb = h % 2
            for c in range(NCH):
                proj_chunk(h, c, kb)
            items = []
            for c in range(NCH):
                def fin(ob, ii, h=h, c=c):
                    k = (h * NCH + c) % 2
                    r0 = (h % 2) * 64
                    return finalize_head(ob, ii, SZh[r0:r0 + 64, c * 512:(c + 1) * 512], [("SZh", c)], rdrow, u, ogt[k], ("ogt", k),
                                         ogscr_d[0, h // 2, (h % 2) * 64:(h % 2) * 64 + 64, c * 512:(c + 1) * 512], ("ogscr", 0, h // 2, c, h % 2))
                items.append(dict(
                    c=c, R=96,
                    Kt=lambda j, kb=kb: Kh[kb][0:96, j * 128:(j + 1) * 128],
                    Q=lambda lo, hi: Qh[0:96, lo:hi],
                    V=lambda j, kb=kb: Vh[kb][:, j, 0:65],
                    rK=lambda j, kb=kb: [("K", kb, j // 4, 0), ("K", kb, j // 4, 1)],
                    rQ=[("Q", c, 0), ("Q", c, 1)],
                    rV=lambda j, kb=kb: [("V", kb, j // 4), ("V1", kb)],
                    fin=fin, pre=None))
            run_attention(items, PT, GEN, 96 ** -0.5)


    def phase_B2(b):
        S.fence()
        PB = Alloc(PHASE0, LIMIT)
        tabc, tabs, mark = build_tables(b, 0, PB)
        PB.off = mark
        QA = [PB.get(SEQ, BF16) for _ in range(2)]
        KA = [PB.get(SEQ, BF16) for _ in range(2)]
        VA = [PB.get([NT, 65], BF16) for _ in range(2)]
        SZ = PB.get(SEQ, BF16)
        wq2 = [PB.get([8, 128], BF16) for _ in range(2)]
        wk2 = [PB.get([8, 128], BF16) for _ in range(2)]
        wv2 = [PB.get([8, 128], BF16) for _ in range(2)]
        wz2 = [PB.get([8, 128], BF16) for _ in range(2)]
        MB = PB.get([NT, 80], F32)
        gm = [PB.get([NT, 16], F32) for _ in range(2)]
        top8 = [PB.get([NT, 8], F32) for _ in range(2)]
        selb = PB.get([NT, 16], F32)
        ksum = [PB.get(16, F32) for _ in range(2)]
        kmh = [PB.get(16, BF16) for _ in range(2)]
        kml = [PB.get(16, BF16) for _ in range(2)]
        PT = [PB.get(512, BF16) for _ in range(4)]
        qpb = PB.get(512, BF16)
        t1 = PB.get(512, F32)
        t2 = PB.get(512, F32)
        th = PB.get(512, F32)
        rdrow = [PB.get(512, BF16) for _ in range(3)]
        u = [PB.get(512, F32) for _ in range(3)]
        ogt = [PB.get(512, BF16) for _ in range(2)]
        S.fence()
        GEN = [5, 6, 7]

        memset("dve", MB.rearrange("p a b -> p (a b)"), 0.0, ["MB"])
        for i in range(2):
            memset("dve", VA[i][:, :, 64:65], 1.0, [("V1", i)])
            dma("sp", KA[i][64:80, :], eind_d, (), [("KE", i)])

        def load_pair_w(p):
            i = p % 2
            dma("pool", wq2[i], win_v[:, :, C_QMB + p * 128:C_QMB + (p + 1) * 128], (), [("wq", i)])
            dma("pool", wk2[i], win_v[:, :, C_KMB + p * 128:C_KMB + (p + 1) * 128], (), [("wk", i)])
            dma("pool", wv2[i], win_v[:, :, C_VMB + p * 128:C_VMB + (p + 1) * 128], (), [("wv", i)])
            dma("pool", wz2[i], win_v[:, :, C_ZMB + p * 128:C_ZMB + (p + 1) * 128], (), [("wz", i)])

        load_pair_w(0)
        for p in range(4):
            if p + 1 < 4:
                load_pair_w(p + 1)
            wq, wk, wv, wz = wq2[p % 2], wk2[p % 2], wv2[p % 2], wz2[p % 2]
            wqk, wkk, wvk, wzk = ("wq", p % 2), ("wk", p % 2), ("wv", p % 2), ("wz", p % 2)
            for c in range(NCH):
                cs = slice(c * 512, (c + 1) * 512)
                hk = hT_keys(c)
                for (w_, wkey, dst, dkey) in ((wq, wqk, QA, "QAd"), (wk, wkk, KA, "KAd")):
                    g0 = gen_rr[0] % 8; g1 = (gen_rr[0] + 1) % 8
                    gen_rr[0] += 2
                    for k in range(8):
                        mm(ps[g0][:, :], w_[:, k, :], hT[:, k, cs], k == 0, k == 7, [wkey] + hk, [PS(g0)])
                    cp("act", qpb, ps[g0][:, :], [PS(g0)], ["qpb"])
                    mm(ps[g1][:, :], rm64, qpb, True, True, ["rm64", "qpb"], [PS(g1)])
                    tt("dve", t1, ps[g0][:, :], tabc[:, cs], ALU.mult, [PS(g0), ("tab", c)], ["t1"])
                    tt("dve", t2, ps[g1][:, :], tabs[:, cs], ALU.mult, [PS(g1), ("tab", c)], ["t2"])
                    tt("dve", dst[0][0:64, cs], t1[0:64, :], t2[0:64, :], ALU.add, ["t1", "t2"], [(dkey, 0, c)])
                    tt("dve", dst[1][0:64, cs], t1[64:128, :], t2[64:128, :], ALU.add, ["t1", "t2"], [(dkey, 1, c)])
                g0 = gen_rr[0] % 8; g1 = (gen_rr[0] + 1) % 8
                gen_rr[0] += 2
                for i in range(4):
                    tsl = slice(c * 512 + i * 128, c * 512 + (i + 1) * 128)
                    for k in range(8):
                        mm(ps[g0][:, i * 128:(i + 1) * 128], hT[:, k, tsl], wv[:, k, :], k == 0, k == 7, [wvk] + hk, [PS(g0)])
                pv = ps[g0][:, :].rearrange("p (i h d) -> p i h d", i=4, h=2)
                cp("act", VA[0][:, 4 * c:4 * c + 4, 0:64], pv[:, :, 0, :], [PS(g0)], [("V", 0, c)])
                cp("act", VA[1][:, 4 * c:4 * c + 4, 0:64], pv[:, :, 1, :], [PS(g0)], [("V", 1, c)])
                for k in range(8):
                    mm(ps[g1][:, :], wz[:, k, :], hT[:, k, cs], k == 0, k == 7, [wzk] + hk, [PS(g1)])
                act(th, ps[g1][:, :], AF.Tanh, [PS(g1)], ["th"], scale=0.5)
                stt("dve", SZ[:, cs], th, 1.0, ps[g1][:, :], ALU.add, ALU.mult, ["th", PS(g1)], [("SZ", c)])

            qkeys = [[("QAd", hh, c) for c in range(NCH)] for hh in range(2)]
            kkeys = [[("KAd", hh, c) for c in range(NCH)] for hh in range(2)]
            gbs = [gen_rr[0] % 8, (gen_rr[0] + 1) % 8]
            gen_rr[0] += 2
            for hh in range(2):
                S.add("dve", lambda e, hh=hh: e.tensor_reduce(out=ksum[hh][0:64, :], in_=KA[hh][0:64, :].rearrange("p (n k) -> p n k", n=16),
                                                             axis=AX.X, op=ALU.add), kkeys[hh], [("ksum", hh)])
            for hh in range(2):
                ts("dve", kmh[hh][0:64, :], ksum[hh][0:64, :], 1.0 / 256, None, ALU.mult, None, [("ksum", hh)], [("kmh", hh)])
                stt("dve", kml[hh][0:64, :], ksum[hh][0:64, :], 1.0 / 256, kmh[hh][0:64, :], ALU.mult, ALU.subtract,
                    [("ksum", hh), ("kmh", hh)], [("kml", hh)])
            for hh in range(2):
                gb = gbs[hh]
                for qt in range(NT):
                    mm(ps[gb][:, qt * 16:(qt + 1) * 16], QA[hh][0:64, qt * 128:(qt + 1) * 128], kmh[hh][0:64, :], True, False,
                       qkeys[hh] + [("kmh", hh)], [PS(gb)])
                    mm(ps[gb][:, qt * 16:(qt + 1) * 16], QA[hh][0:64, qt * 128:(qt + 1) * 128], kml[hh][0:64, :], False, True,
                       qkeys[hh] + [("kml", hh)], [PS(gb)])
            for hh in range(2):
                tt("dve", gm[hh].rearrange("p a b -> p (a b)"), ps[gbs[hh]][:, :], cb.rearrange("p a b -> p (a b)"), ALU.add,
                   [PS(gbs[hh]), "cb"], [("gm", hh)])
            for hh in range(2):
                for qt in range(NT):
                    S.add("dve", lambda e, qt=qt, hh=hh: e.max(out=top8[hh][:, qt, :], in_=gm[hh][:, qt, :]), [("gm", hh)], [("top8", hh, qt)])
            for hh in range(2):
                tt("dve", selb, gm[hh], top8[hh][:, :, 3:4].to_broadcast([128, NT, 16]), ALU.is_ge,
                   [("gm", hh)] + [("top8", hh, qt) for qt in range(NT)], ["selb"])
                ts("dve", MB[:, :, 64:80], selb, -1.0, -NEG, ALU.add, ALU.mult, ["selb", "MB"], ["MBd"])
                for grp in range(NCH):
                    g0 = gen_rr[0] % 8
                    gen_rr[0] += 1
                    for i in range(4):
                        qt = grp * 4 + i
                        tr(ps[g0][0:80, i * 128:(i + 1) * 128], MB[:, qt, :], ident_f, ["MBd", "ident_f"], [PS(g0)])
                    cp("act", QA[hh][64:80, grp * 512:(grp + 1) * 512], ps[g0][64:80, :], [PS(g0)], [("QAm", hh, grp)])

            items = []
            for hh in range(2):
                h = 2 * p + hh
                for c in range(NCH):
                    def fin(ob, ii, h=h, hh=hh, c=c):
                        k = (h * NCH + c) % 2
                        return finalize_head(ob, ii, SZ[hh * 64:hh * 64 + 64, c * 512:(c + 1) * 512], [("SZ", c)], rdrow, u, ogt[k], ("ogt", k),
                                      ogscr_d[1, h // 2, (h % 2) * 64:(h % 2) * 64 + 64, c * 512:(c + 1) * 512], ("ogscr", 1, h // 2, c, h % 2))
                    items.append(dict(
                        c=c, R=80,
                        Kt=lambda j, hh=hh: KA[hh][0:80, j * 128:(j + 1) * 128],
                        Q=lambda lo, hi, hh=hh: QA[hh][0:80, lo:hi],
                        V=lambda j, hh=hh: VA[hh][:, j, 0:65],
                        rK=lambda j, hh=hh: [("KAd", hh, j // 4), ("KE", hh)],
                        rQ=[("QAd", hh, c), ("QAm", hh, c)],
                        rV=lambda j, hh=hh: [("V", hh, j // 4), ("V1", hh)],
                        fin=fin, pre=None))
            run_attention(items, PT, GEN, 64 ** -0.5)


    def phase_C(b):
        S.fence()
        PB = Alloc(PHASE0, LIMIT)
        wm = PB.get([8, 2048], BF16)
        woa = PB.get([4, DM], BF16)
        wob = PB.get([4, DM], BF16)
        wout = PB.get([8, DM], BF16)
        oga = [PB.get([4, 512], BF16) for _ in range(2)]
        ogb = [PB.get([4, 512], BF16) for _ in range(2)]
        ta = [PB.get(512, F32) for _ in range(2)]
        tb = [PB.get(512, F32) for _ in range(2)]
        m1 = [PB.get(512, F32) for _ in range(2)]
        m2 = [PB.get(512, F32) for _ in range(2)]
        ymT = PB.get([8, 512], BF16)
        crr = [0]
        xt2 = [PB.get(DM, F32) for _ in range(2)]
        ot = [PB.get(DM, F32) for _ in range(2)]
        junk = PB.get(DM, BF16)
        ssy = PB.get(4, F32)
        S.fence()
        dma("pool", wm, win_v[:, :, C_MERGE:D_IN], (), ["wm"])
        dma("pool", woa, womla_d.rearrange("(k p) c -> p k c", p=128), (), ["woa"])
        dma("pool", wob, womb_d.rearrange("(k p) c -> p k c", p=128), (), ["wob"])
        dma("pool", wout, wout_d.rearrange("(k p) c -> p k c", p=128), (), ["wout"])
        def load_og(c):
            cs_ = slice(c * 512, (c + 1) * 512)
            oi_ = c % 2
            for pp in range(4):
                dma("sp", oga[oi_][:, pp, :], ogscr_d[0, pp, :, cs_], [("ogscr", 0, pp, c, 0), ("ogscr", 0, pp, c, 1)], [("oga", oi_)])
                dma("sp", ogb[oi_][:, pp, :], ogscr_d[1, pp, :, cs_], [("ogscr", 1, pp, c, 0), ("ogscr", 1, pp, c, 1)], [("ogb", oi_)])

        load_og(0)
        for c in range(NCH):
            cs = slice(c * 512, (c + 1) * 512)
            hk = hT_keys(c)
            oi = c % 2
            if c + 1 < NCH:
                load_og(c + 1)
            for d in range(8):
                ds_ = slice(d * 128, (d + 1) * 128)
                b0_, b1_, b2_, b3_ = [(crr[0] + i) % 8 for i in range(4)]
                crr[0] += 4
                for k in range(8):
                    mm(ps[b0_][:, :], wm[:, k, d * 128:(d + 1) * 128], hT[:, k, cs], k == 0, k == 7, ["wm"] + hk, [PS(b0_)])
                for k in range(8):
                    mm(ps[b1_][:, :], wm[:, k, DM + d * 128:DM + (d + 1) * 128], hT[:, k, cs], k == 0, k == 7, ["wm"] + hk, [PS(b1_)])
                for k in range(4):
                    mm(ps[b2_][:, :], woa[:, k, ds_], oga[oi][:, k, :], k == 0, k == 3, ["woa", ("oga", oi)], [PS(b2_)])
                for k in range(4):
                    mm(ps[b3_][:, :], wob[:, k, ds_], ogb[oi][:, k, :], k == 0, k == 3, ["wob", ("ogb", oi)], [PS(b3_)])
                di = d % 2
                act(ta[di], ps[b0_][:, :], AF.Tanh, [PS(b0_), "bmh"], [("ta", di)], scale=0.5, bias=bmh[:, d:d + 1])
                act(tb[di], ps[b1_][:, :], AF.Tanh, [PS(b1_), "bmh"], [("tb", di)], scale=0.5, bias=bmh[:, 8 + d:9 + d])
                stt("dve", m1[di], ta[di], 1.0, ps[b2_][:, :], ALU.add, ALU.mult, [("ta", di), PS(b2_)], [("m1", di)])
                stt("dve", m2[di], tb[di], 1.0, ps[b3_][:, :], ALU.add, ALU.mult, [("tb", di), PS(b3_)], [("m2", di)])
                tt("pool", ymT[:, d, :], m1[di], m2[di], ALU.add, [("m1", di), ("m2", di)], [("ymT", d)])
            for i in range(4):
                t = 4 * c + i
                xi = t % 2
                dma("sp", xt2[xi], x_d[b, t * 128:(t + 1) * 128, :], (), [("xt2", xi)])
                pb = crr[0] % 8
                crr[0] += 2
                if pb == 7:
                    pb = 0
                    crr[0] += 1
                for half in range(2):
                    for k in range(8):
                        mm(ps[pb + half][:, :], ymT[:, k, i * 128:(i + 1) * 128], wout[:, k, half * 512:(half + 1) * 512],
                           k == 0, k == 7, ["wout", ("ymT", k)], [PS(pb + half)])
                memset("dve", ssy[:, 0:2], 0.0, [("ssy", 0), ("ssy", 1), "ssyz"])
                for half in range(2):
                    act(junk[:, half * 512:(half + 1) * 512], ps[pb + half][:, :], AF.Square, [PS(pb + half), "ssyz"], ["junk", ("ssy", half)],
                        accum_out=ssy[:, half:half + 1])
                tt("dve", ssy[:, 2:3], ssy[:, 0:1], ssy[:, 1:2], ALU.add, [("ssy", 0), ("ssy", 1)], ["ssy2"])
                act(ssy[:, 3:4], ssy[:, 2:3], AF.Sqrt, ["ssy2", "eps_t"], ["ssy3"], scale=1.0 / DM, bias=eps4_ap)
                recip(ssy[:, 2:3], ssy[:, 3:4], ["ssy3"], ["ssy2"])
                for half in range(2):
                    hs = slice(half * 512, (half + 1) * 512)
                    stt("dve", ot[xi][:, hs], ps[pb + half][:, :], ssy[:, 2:3], G2[:, b, hs], ALU.mult, ALU.mult,
                        [PS(pb + half), "ssy2", ("G2", b)], [("ot", xi)])
                tt("pool", ot[xi], ot[xi], xt2[xi], ALU.add, [("ot", xi), ("xt2", xi)], [("ot", xi)])
                dma("pool", out_d[b, t * 128:(t + 1) * 128, :], ot[xi], [("ot", xi)], [("outst", b, t)])

    if debug == "0":
        d_A = dbg("A", [128, NS * 8]); d_B = dbg("B", [128, NS * 8]); d_G = dbg("G2", [128, NS * DM])
        dma("sp", d_A, A_sb.rearrange("p a b -> p (a b)"), [("AB", b) for b in range(NS)], [("dbg", 0)])
        dma("sp", d_B, B_sb.rearrange("p a b -> p (a b)"), [("AB", b) for b in range(NS)], [("dbg", 1)])
        dma("sp", d_G, G2.rearrange("p a b -> p (a b)"), [("G2", b) for b in range(NS)], [("dbg", 2)])
    for b in range(NS if debug != "0" else 0):
        phase_A(b)
        if debug == "A":
            d_h = dbg("hT%d" % b, [8, 128, SEQ], BF16)
            for d in range(8):
                dma("sp", d_h[d], hT[:, d, :], [k for c in range(NCH) for k in hT_keys(c)], [("dbg", b, d)])
            continue
        phase_B1(b)
        if debug == "B1":
            d_o = dbg("ogmla%d" % b, [4, 128, SEQ], BF16)
            S.fence()
            for pp in range(4):
                dma("sp", d_o[pp], ogscr_d[0, pp], (), [("dbg", b, pp)])
            continue
        if debug == "B2only":
            pass
        phase_B2(b)
        if debug == "B2":
            d_o = dbg("ogmb%d" % b, [4, 128, SEQ], BF16)
            S.fence()
            for pp in range(4):
                dma("sp", d_o[pp], ogscr_d[1, pp], (), [("dbg", b, pp)])
            continue
        phase_C(b)

    S.add("sp", None, r=[k for k in list(S.last_w.keys()) if isinstance(k, tuple) and k and k[0] in ("dbg", "outst")], w=())
    S.finalize()
    sems = {}
    for s, n in S.counts.items():
        nsem = 1 if s[0] == "dma" else (n - 1) // S.PER_SEM + 1
        if s[0] == "dma":
            assert n * 16 < 60000, (s, n)
        nm = "_".join(str(v) for v in s)
        sems[s] = [es.enter_context(nc.semaphore("sem_%s_%d" % (nm, k))) for k in range(nsem)]
    with nc.allow_low_precision(reason="bf16 matmul operands by design (fp32 PSUM accumulation)"), nc.Block() as block:
        S.emit(nc, block, sems)
    es.close()
    return nc, dbg_out


def _prep_inputs(inputs, NS, core):
    f32 = np.float32
    b0 = core * NS
    g = lambda k: np.asarray(inputs[k])
    m = {}
    m["x"] = np.ascontiguousarray(g("x")[b0:b0 + NS]).astype(f32, copy=False)
    m["pos"] = np.ascontiguousarray(g("positions")[b0:b0 + NS]).astype(np.int32, copy=False)
    c = g("c")[b0:b0 + NS].astype(f32)
    cT = c.reshape(NS, 8, 128).transpose(2, 1, 0)
    m["cT"] = np.ascontiguousarray(cT.reshape(128, 8 * NS))
    m["w_ada"] = np.ascontiguousarray(g("w_ada")[0], dtype=f32)
    bada = g("b_ada")[0].astype(f32)
    m["badaT"] = _T(bada, 24)
    m["badag_bc"] = np.ascontiguousarray(np.broadcast_to(bada[2048:3072][None, :], (128, DM)))
    m["gpreT"] = _T(g("g_pre")[0], 8)
    m["gpost_bc"] = np.ascontiguousarray(np.broadcast_to(g("g_post")[0].astype(f32)[None, :], (128, DM)))
    m["w_in"] = np.ascontiguousarray(g("w_in")[0], dtype=f32)
    m["gqT"] = _T(g("g_q_lat")[0], 2)
    m["gkvT"] = _T(g("g_kv_lat")[0], 1)
    m["w_uq"] = np.ascontiguousarray(g("w_uq")[0], dtype=f32)
    m["w_ukv"] = np.ascontiguousarray(g("w_ukv")[0], dtype=f32)
    m["w_o_mla"] = np.ascontiguousarray(g("w_o_mla")[0], dtype=f32)
    m["w_o_mb"] = np.ascontiguousarray(g("w_o_moba")[0], dtype=f32)
    m["bmT"] = _T(g("b_merge")[0], 16)
    m["w_out"] = np.ascontiguousarray(g("w_out")[0], dtype=f32)
    m.update(_consts())
    return m


def kernel(**inputs):
    NS = 2
    nc, _ = build(NS=NS)
    in_maps = [_prep_inputs(inputs, NS, core) for core in range(NCORES)]
    res = run_bass_kernel_spmd(nc, in_maps, core_ids=list(range(NCORES)))
    out = np.concatenate([np.asarray(r["out"]) for r in res.results], axis=0)
    return out.astype(np.float32, copy=False)
```
